# Optimizing a Trainium2 kernel written in Bass

```python
import math
import jax, jax.numpy as jnp
from jax import lax
import numpy as np

D_MODEL = 1024
BATCH = 8
SEQ = 4096
DEPTH = 1

N_Q_HEADS = 16
N_KV_HEADS = 4
HEAD_DIM = 64
Q_PER_KV = N_Q_HEADS // N_KV_HEADS
ATTN_WIDTH = N_Q_HEADS * HEAD_DIM
KV_WIDTH = N_KV_HEADS * HEAD_DIM
WINDOW = 128
ATTN_BLOCK = 128
ROT_DIM = HEAD_DIM // 4
ROPE_THETA = 500000.0

SSD_WIDTH = 2 * D_MODEL
SSD_HEAD_DIM = 64
SSD_HEADS = SSD_WIDTH // SSD_HEAD_DIM
SSD_GROUPS = 4
SSD_HEADS_PER_GROUP = SSD_HEADS // SSD_GROUPS
SSD_STATE = 128
SSD_CONV = 4
SSD_CHUNK = 128
CONV_DIM = SSD_WIDTH + 2 * SSD_GROUPS * SSD_STATE

N_BRANCHES = 2
GATE_WIDTH = N_BRANCHES * D_MODEL

IN_SPLITS = [ATTN_WIDTH, ATTN_WIDTH + KV_WIDTH, ATTN_WIDTH + 2 * KV_WIDTH,
             ATTN_WIDTH + 2 * KV_WIDTH + SSD_WIDTH,
             ATTN_WIDTH + 2 * KV_WIDTH + SSD_WIDTH + CONV_DIM,
             ATTN_WIDTH + 2 * KV_WIDTH + SSD_WIDTH + CONV_DIM + SSD_HEADS]
IN_DIM = IN_SPLITS[-1] + GATE_WIDTH

PEER_HEADS = 8
PEER_N_KEYS = 128
PEER_N_EXPERTS = PEER_N_KEYS * PEER_N_KEYS
PEER_TOPK = 16
PEER_QUERY_DIM = 256
PEER_HALF = PEER_QUERY_DIM // 2
PEER_TOKEN_BLOCK = 128

EPS = 1e-6

kernel_name = "hybrid_swa_ssd_peer_block"


def rms_norm(x, w):
    xf = x.astype(jnp.float32)
    y = xf * lax.rsqrt(jnp.mean(xf * xf, axis=-1, keepdims=True) + EPS)
    return (y * w.astype(jnp.float32)).astype(x.dtype)


def partial_rope(t, positions):
    half = ROT_DIM // 2
    inv_freq = ROPE_THETA ** (-jnp.arange(0, ROT_DIM, 2, dtype=jnp.float32) / ROT_DIM)
    ang = positions.astype(jnp.float32)[..., None] * inv_freq
    cos = jnp.cos(ang)[:, :, None, :]
    sin = jnp.sin(ang)[:, :, None, :]
    tf = t.astype(jnp.float32)
    x1 = tf[..., :half]
    x2 = tf[..., half:ROT_DIM]
    rot = jnp.concatenate([x1 * cos - x2 * sin, x2 * cos + x1 * sin], axis=-1)
    return jnp.concatenate([rot.astype(t.dtype), t[..., ROT_DIM:]], axis=-1)


def sliding_window_attention(q, k, v, sinks):
    B, S = q.shape[0], q.shape[1]
    Q = ATTN_BLOCK
    nb = S // Q
    qb = jnp.moveaxis(q.reshape(B, nb, Q, N_KV_HEADS, Q_PER_KV, HEAD_DIM), 1, 0)

    def windows(t):
        tb = t.reshape(B, nb, Q, N_KV_HEADS, HEAD_DIM)
        prev = jnp.concatenate([jnp.zeros_like(tb[:, :1]), tb[:, :-1]], axis=1)
        return jnp.moveaxis(jnp.concatenate([prev, tb], axis=2), 1, 0)

    kw = windows(k)
    vw = windows(v)
    sink = sinks.astype(jnp.float32).reshape(N_KV_HEADS, Q_PER_KV)[None, :, :, None, None]
    qi = jnp.arange(Q)[:, None]
    kj = jnp.arange(2 * Q)[None, :]
    rel = Q + qi - kj
    band = (rel >= 0) & (rel < WINDOW)
    scale = HEAD_DIM ** -0.5

    def block(args):
        qblk, kblk, vblk, bidx = args
        s = jnp.einsum("bqhgd,bkhd->bhgqk", qblk, kblk).astype(jnp.float32) * scale
        valid = band & (bidx * Q + kj - Q >= 0)
        s = jnp.where(valid, s, -jnp.inf)
        m = jnp.maximum(jnp.max(s, axis=-1, keepdims=True), sink)
        p = jnp.exp(s - m)
        p = p / (jnp.sum(p, axis=-1, keepdims=True) + jnp.exp(sink - m))
        return jnp.einsum("bhgqk,bkhd->bqhgd", p.astype(vblk.dtype), vblk)

    out = lax.map(block, (qb, kw, vw, jnp.arange(nb)))
    return jnp.moveaxis(out, 0, 1).reshape(B, S, ATTN_WIDTH)


def causal_depthwise_conv(u, w, b):
    C = u.shape[-1]
    out = lax.conv_general_dilated(
        u, w[:, None, :].astype(u.dtype), window_strides=(1,),
        padding=[(SSD_CONV - 1, 0)], dimension_numbers=("NWC", "WIO", "NWC"),
        feature_group_count=C)
    return out + b.astype(u.dtype)


def ssd_chunked_scan(xdt, dA, Bm, Cm):
    B, S = xdt.shape[0], xdt.shape[1]
    L = SSD_CHUNK
    nc = S // L
    G, R, P, N = SSD_GROUPS, SSD_HEADS_PER_GROUP, SSD_HEAD_DIM, SSD_STATE
    x_c = jnp.moveaxis(xdt.reshape(B, nc, L, G, R, P), 1, 0)
    a_c = jnp.moveaxis(dA.reshape(B, nc, L, G, R), 1, 0)
    b_c = jnp.moveaxis(Bm.reshape(B, nc, L, G, N), 1, 0)
    c_c = jnp.moveaxis(Cm.reshape(B, nc, L, G, N), 1, 0)
    causal = jnp.tril(jnp.ones((L, L), dtype=bool))[None, :, :, None, None]

    def step(state, inp):
        xc, ac, bc, cc = inp
        acum = jnp.cumsum(ac, axis=1)
        seg = acum[:, :, None] - acum[:, None, :]
        decay = jnp.exp(jnp.where(causal, seg, -jnp.inf))
        cb = jnp.einsum("btgn,bsgn->btsg", cc, bc)
        y = jnp.einsum("btsgr,bsgrp->btgrp", cb[..., None] * decay, xc)
        y = y + jnp.einsum("btgn,bgrpn->btgrp", cc, state) * jnp.exp(acum)[..., None]
        total = acum[:, -1]
        w = jnp.exp(total[:, None] - acum)
        state = state * jnp.exp(total)[..., None, None] + jnp.einsum(
            "bsgn,bsgrp->bgrpn", bc, w[..., None] * xc)
        return state, y

    state0 = jnp.zeros((B, G, R, P, N), dtype=jnp.float32)
    _, ys = lax.scan(step, state0, (x_c, a_c, b_c, c_c))
    return jnp.moveaxis(ys, 0, 1).reshape(B, S, SSD_HEADS, SSD_HEAD_DIM)


def peer_ffn(h, wq, pkeys, u_tab, v_tab):
    B, S, D = h.shape
    T = B * S
    hf = h.reshape(T, D)
    q = (hf @ wq).reshape(T, PEER_HEADS, 2, PEER_HALF)
    s = jnp.einsum("thcd,hckd->thck", q, pkeys).astype(jnp.float32)
    v1, i1 = lax.top_k(s[:, :, 0], PEER_TOPK)
    v2, i2 = lax.top_k(s[:, :, 1], PEER_TOPK)
    cand = (v1[..., :, None] + v2[..., None, :]).reshape(T, PEER_HEADS, PEER_TOPK * PEER_TOPK)
    sc, flat = lax.top_k(cand, PEER_TOPK)
    e1 = jnp.take_along_axis(i1, flat // PEER_TOPK, axis=-1)
    e2 = jnp.take_along_axis(i2, flat % PEER_TOPK, axis=-1)
    idx = e1 * PEER_N_KEYS + e2
    g = jax.nn.softmax(sc, axis=-1).astype(h.dtype)
    nblk = T // PEER_TOKEN_BLOCK
    HK = PEER_HEADS * PEER_TOPK
    hb = hf.reshape(nblk, PEER_TOKEN_BLOCK, D)
    ib = idx.reshape(nblk, PEER_TOKEN_BLOCK, HK)
    gb = g.reshape(nblk, PEER_TOKEN_BLOCK, HK)

    def block(args):
        hx, ix, gx = args
        ue = jnp.take(u_tab, ix, axis=0)
        a = jnp.einsum("td,tkd->tk", hx, ue)
        ve = jnp.take(v_tab, ix, axis=0)
        return jnp.einsum("tk,tkd->td", gx * jax.nn.gelu(a, approximate=False), ve)

    out = lax.map(block, (hb, ib, gb))
    return out.reshape(B, S, D)


def setup_inputs(seed: int = 0) -> dict:
    key = jax.random.key(seed)
    ks = jax.random.split(key, 20)
    f32 = jnp.float32
    nrm = lambda k, shape, sc: jax.random.normal(k, shape, f32) * sc
    x = jax.random.normal(ks[0], (BATCH, SEQ, D_MODEL), f32)
    positions = jnp.broadcast_to(jnp.arange(SEQ, dtype=jnp.int32), (BATCH, SEQ))
    dt = jnp.exp(jax.random.uniform(ks[7], (DEPTH, SSD_HEADS), f32,
                                    minval=math.log(1e-3), maxval=math.log(1e-1)))
    dt_bias = dt + jnp.log(-jnp.expm1(-dt))
    a_log = jnp.log(jax.random.uniform(ks[8], (DEPTH, SSD_HEADS), f32, minval=1.0, maxval=16.0))
    return {
        "x": x,
        "positions": positions,
        "norm_mix_w": 1.0 + nrm(ks[1], (DEPTH, D_MODEL), 0.02),
        "w_in": nrm(ks[2], (DEPTH, D_MODEL, IN_DIM), D_MODEL ** -0.5),
        "attn_sinks": nrm(ks[3], (DEPTH, N_Q_HEADS), 0.5),
        "conv_w": nrm(ks[4], (DEPTH, SSD_CONV, CONV_DIM), SSD_CONV ** -0.5),
        "conv_b": nrm(ks[5], (DEPTH, CONV_DIM), 0.01),
        "dt_bias": dt_bias,
        "a_log": a_log,
        "d_skip": 1.0 + nrm(ks[6], (DEPTH, SSD_HEADS), 0.02),
        "ssd_norm_w": 1.0 + nrm(ks[9], (DEPTH, SSD_WIDTH), 0.02),
        "w_attn_o": nrm(ks[10], (DEPTH, ATTN_WIDTH, D_MODEL), ATTN_WIDTH ** -0.5),
        "w_ssd_o": nrm(ks[11], (DEPTH, SSD_WIDTH, D_MODEL), SSD_WIDTH ** -0.5),
        "w_out": nrm(ks[12], (DEPTH, D_MODEL, D_MODEL), D_MODEL ** -0.5),
        "norm_ffn_w": 1.0 + nrm(ks[13], (DEPTH, D_MODEL), 0.02),
        "peer_wq": nrm(ks[14], (DEPTH, D_MODEL, PEER_HEADS * PEER_QUERY_DIM), D_MODEL ** -0.5),
        "peer_keys": nrm(ks[15], (DEPTH, PEER_HEADS, 2, PEER_N_KEYS, PEER_HALF), PEER_HALF ** -0.5),
        "peer_u": nrm(ks[16], (DEPTH, PEER_N_EXPERTS, D_MODEL), D_MODEL ** -0.5),
        "peer_v": nrm(ks[17], (DEPTH, PEER_N_EXPERTS, D_MODEL), PEER_HEADS ** -0.5),
        "norm_final_w": 1.0 + nrm(ks[18], (D_MODEL,), 0.02),
    }


def reference(x, positions, norm_mix_w, w_in, attn_sinks, conv_w, conv_b, dt_bias, a_log,
              d_skip, ssd_norm_w, w_attn_o, w_ssd_o, w_out, norm_ffn_w, peer_wq, peer_keys,
              peer_u, peer_v, norm_final_w):
    B, S, _ = x.shape
    f32 = jnp.float32
    for l in range(DEPTH):
        h = rms_norm(x, norm_mix_w[l])
        proj = h @ w_in[l]
        q, k, v, z, xbc, dt_raw, gates = jnp.split(proj, IN_SPLITS, axis=-1)

        q = partial_rope(q.reshape(B, S, N_Q_HEADS, HEAD_DIM), positions)
        k = partial_rope(k.reshape(B, S, N_KV_HEADS, HEAD_DIM), positions)
        v = v.reshape(B, S, N_KV_HEADS, HEAD_DIM)
        attn = sliding_window_attention(q, k, v, attn_sinks[l]) @ w_attn_o[l]

        xbc = jax.nn.silu(causal_depthwise_conv(xbc, conv_w[l], conv_b[l]))
        xs, bm, cm = jnp.split(xbc, [SSD_WIDTH, SSD_WIDTH + SSD_GROUPS * SSD_STATE], axis=-1)
        xs = xs.reshape(B, S, SSD_HEADS, SSD_HEAD_DIM).astype(f32)
        bm = bm.reshape(B, S, SSD_GROUPS, SSD_STATE).astype(f32)
        cm = cm.reshape(B, S, SSD_GROUPS, SSD_STATE).astype(f32)
        dt = jax.nn.softplus(dt_raw.astype(f32) + dt_bias[l].astype(f32))
        a = -jnp.exp(a_log[l].astype(f32))
        y = ssd_chunked_scan(xs * dt[..., None], dt * a, bm, cm)
        y = y + d_skip[l].astype(f32)[:, None] * xs
        y = y.reshape(B, S, SSD_WIDTH) * jax.nn.silu(z.astype(f32))
        y = rms_norm(y.reshape(B, S, SSD_GROUPS, SSD_WIDTH // SSD_GROUPS),
                     ssd_norm_w[l].reshape(SSD_GROUPS, SSD_WIDTH // SSD_GROUPS))
        ssd = y.reshape(B, S, SSD_WIDTH).astype(x.dtype) @ w_ssd_o[l]

        g_attn, g_ssd = jnp.split(jax.nn.sigmoid(gates), N_BRANCHES, axis=-1)
        x = x + (g_attn * attn + g_ssd * ssd) @ w_out[l]

        x = x + peer_ffn(rms_norm(x, norm_ffn_w[l]), peer_wq[l], peer_keys[l], peer_u[l], peer_v[l])
    return rms_norm(x, norm_final_w)
```

```python
import numpy as np
import ml_dtypes
from contextlib import ExitStack
import concourse.bass as bass
import concourse.mybir as mybir
from concourse.bass_utils import run_bass_kernel_spmd

F32 = mybir.dt.float32
BF16 = mybir.dt.bfloat16
I32 = mybir.dt.int32
U32 = mybir.dt.uint32
AF = mybir.ActivationFunctionType
ALU = mybir.AluOpType
AX = mybir.AxisListType

D = 1024
SEQ = 4096
NEG = -30000.0
EPS = 1e-6
IN_DIM = 8736
TMW = 5664
ENGS = ["pe", "act", "dve", "pool", "sp"]
ARENA_WORDS = 45040


class Prog:
    def __init__(self, nc):
        self.nc = nc
        self.ops = {e: [] for e in ENGS}
        self.lastw = {}
        self.readers = {}
        self.dma_cnt = {}
        self.fence = {e: None for e in ENGS}

    def op(self, eng, fn, reads=(), writes=(), dma=False, semkey=None):
        idx = len(self.ops[eng])
        raw, other = set(), set()
        for k in reads:
            raw.update(self.lastw.get(k, {}).values())
        for k in writes:
            raw.update(self.lastw.get(k, {}).values())
            other.update(self.readers.get(k, {}).values())
        deps = set()
        for d in raw | other:
            de, di = d
            drec = self.ops[de][di]
            if de == eng and not drec["dma"] and not dma:
                if eng == "pe" or d not in raw:
                    continue
            deps.add(d)
        rec = dict(fn=fn, deps=deps, dma=dma, needed=False, inc=None, fence=self.fence[eng])
        self.fence[eng] = None
        if dma:
            c = self.dma_cnt.get(semkey, 0) + 1
            self.dma_cnt[semkey] = c
            rec["semkey"] = semkey
            rec["dma_val"] = 16 * c
            tk = ("d", eng, semkey)
        else:
            tk = ("c", eng)
        self.ops[eng].append(rec)
        me = (eng, idx)
        for k in reads:
            self.readers.setdefault(k, {})[tk] = me
        for k in writes:
            self.lastw.setdefault(k, {})[tk] = me
        return me

    def barrier(self):
        snap_c = {}
        for e in ENGS:
            for i in range(len(self.ops[e]) - 1, -1, -1):
                if not self.ops[e][i]["dma"]:
                    snap_c[e] = i
                    self.ops[e][i]["needed"] = True
                    break
        snap = (snap_c, dict(self.dma_cnt))
        for e in ENGS:
            self.fence[e] = snap

    def emit(self, out_semkeys=()):
        nc = self.nc
        for e in ENGS:
            for rec in self.ops[e]:
                for (de, di) in rec["deps"]:
                    d = self.ops[de][di]
                    if not d["dma"]:
                        d["needed"] = True
        for e in ENGS:
            c = 0
            for rec in self.ops[e]:
                if rec["needed"] and not rec["dma"]:
                    c += 1
                    rec["inc"] = c
        with ExitStack() as st:
            esem = {e: st.enter_context(nc.semaphore("sem_" + e)) for e in ENGS}
            dsem = {}
            for i, k in enumerate(self.dma_cnt):
                dsem[k] = st.enter_context(nc.semaphore("dsem_%d" % i))
            block = st.enter_context(nc.Block())
            prog = self

            def body(ename):
                def f(eng):
                    waited = {}

                    def w(key, s, v):
                        if waited.get(key, 0) >= v:
                            return
                        waited[key] = v
                        eng.wait_ge(s, v)

                    for rec in prog.ops[ename]:
                        if rec["fence"] is not None:
                            sc, sd = rec["fence"]
                            for fe, fi in sc.items():
                                w(("e", fe), esem[fe], prog.ops[fe][fi]["inc"])
                            for k, cnt in sd.items():
                                w(("d", k), dsem[k], 16 * cnt)
                        for (de, di) in sorted(rec["deps"]):
                            d = prog.ops[de][di]
                            if d["dma"]:
                                w(("d", d["semkey"]), dsem[d["semkey"]], d["dma_val"])
                            else:
                                w(("e", de), esem[de], d["inc"])
                        ins = rec["fn"](eng)
                        if rec["dma"]:
                            ins.then_inc(dsem[rec["semkey"]], 16)
                        elif rec["needed"]:
                            ins.then_inc(esem[ename], 1)
                    if ename == "sp":
                        for k in out_semkeys:
                            eng.wait_ge(dsem[k], 16 * prog.dma_cnt[k])
                return f

            block.tensor(body("pe"))
            block.scalar(body("act"))
            block.vector(body("dve"))
            block.gpsimd(body("pool"))
            block.sync(body("sp"))


class Arena:
    def __init__(self, base_ap, words):
        self.base = base_ap
        self.words = words
        self.off = 0

    def reset(self):
        self.off = 0

    def alloc(self, shape, dtype=F32):
        n = int(np.prod(shape))
        if dtype == BF16:
            w = (n + 1) // 2
        else:
            w = n
        w = (w + 7) // 8 * 8
        a = self.off
        self.off += w
        assert self.off <= self.words, "arena overflow %d > %d" % (self.off, self.words)
        v = self.base[:, a:a + w]
        if dtype != F32:
            v = v.bitcast(dtype)
        v = v[:, 0:n]
        if len(shape) == 2:
            v = v.rearrange("p (a b) -> p a b", b=shape[1])
        elif len(shape) == 3:
            v = v.rearrange("p (a b c) -> p a b c", b=shape[1], c=shape[2])
        return v


def build_program(NT=32, debug=False, phases=("A", "A2", "B1a", "B1b", "B2")):
    T = NT * 128
    nc = bass.Bass("TRN2", target_bir_lowering=False)

    def din(name, shape, dt=F32):
        return nc.dram_tensor(name, list(shape), dt, kind="ExternalInput").ap()

    skind = "ExternalOutput" if debug else "Internal"

    def dscr(name, shape, dt=F32):
        return nc.dram_tensor(name, list(shape), dt, kind=skind).ap()

    x = din("x", [SEQ, D])
    pos = din("pos", [128, 32], I32)
    w_in = din("w_in", [D, IN_DIM])
    w_attn_o = din("w_attn_o", [1024, D])
    w_ssd_o = din("w_ssd_o", [2048, D])
    w_out = din("w_out", [D, D])
    peer_wq = din("peer_wq", [D, 2048])
    keysT = din("keysT", [128, 16 * 128])
    peer_uv = din("peer_uv", [16384, 2 * D])
    nmw = din("nmw", [128, D])
    nfw = din("nfw", [128, D])
    nlw = din("nlw", [128, D])
    snwc = din("snwc", [128, 16])
    sinks = din("sinks", [128, 16])
    convw = din("convw", [128, 24 * 4])
    convb = din("convb", [128, 24])
    dtb = din("dtb", [128, 32])
    alog = din("alog", [128, 32])
    dskip = din("dskip", [128, 32])
    c_ident = din("c_ident", [128, 128], BF16)
    c_identf = din("c_identf", [128, 128])
    c_maskc = din("c_maskc", [128, 512], BF16)
    c_maskp = din("c_maskp", [128, 512], BF16)
    c_U = din("c_U", [128, 128])
    c_L = din("c_L", [128, 128])
    c_ones = din("c_ones", [128, 128])
    c_iota = din("c_iota", [128, 2048], BF16)
    c_invf = din("c_invf", [128, 8])
    out = nc.dram_tensor("out", [SEQ, D], F32, kind="ExternalOutput").ap()
    pt = dscr("pt", [SEQ, TMW])
    xbcT = dscr("xbcT", [3072, SEQ])
    xc = dscr("xc", [3072, SEQ])
    ma = dscr("ma", [SEQ, D])
    x1s = dscr("x1s", [SEQ, D])
    uvb = nc.dram_tensor("uvb", [16384, 2 * D], BF16, kind="Internal").ap()

    st = ExitStack()
    arena_t = st.enter_context(nc.sbuf_tensor("arena", [128, ARENA_WORDS], F32))
    AR = Arena(arena_t[:], ARENA_WORDS)
    PS = [st.enter_context(nc.psum_tensor("ps%d" % i, [128, 512], F32)) for i in range(8)]
    P = Prog(nc)

    def dma(q, out_ap, in_ap, reads, writes, semkey):
        P.op(q, lambda e: e.dma_start(out=out_ap, in_=in_ap), reads=reads, writes=writes,
             dma=True, semkey=semkey)

    def mm(out_ap, lhsT, rhs, start, stop, reads, writes, skip=False):
        P.op("pe", lambda e: e.matmul(out_ap, lhsT, rhs, start=start, stop=stop,
                                      skip_group_check=skip), reads=reads, writes=writes)

    def tr(out_ap, in_ap, ident, reads, writes):
        P.op("pe", lambda e: e.transpose(out_ap, in_ap, ident), reads=reads, writes=writes)

    def act(out_ap, in_ap, func, reads, writes, bias=None, scale=None, accum_out=None):
        kw = {}
        if bias is not None:
            kw["bias"] = bias
        if scale is not None:
            kw["scale"] = scale
        if accum_out is not None:
            kw["accum_out"] = accum_out
        P.op("act", lambda e: e.activation(out_ap, in_ap, func, **kw), reads=reads, writes=writes)

    def tt(eng, out_ap, in0, in1, op, reads, writes):
        P.op(eng, lambda e: e.tensor_tensor(out_ap, in0, in1, op), reads=reads, writes=writes)

    def ts(eng, out_ap, in0, s1, s2, op0, op1, reads, writes, accum_out=None):
        if op1 is None:
            P.op(eng, lambda e: e.tensor_scalar(out_ap, in0, s1, None, op0), reads=reads, writes=writes)
        else:
            P.op(eng, lambda e: e.tensor_scalar(out_ap, in0, s1, s2, op0, op1, accum_out=accum_out),
                 reads=reads, writes=writes)

    def stt(out_ap, in0, scalar, in1, op0, op1, reads, writes, accum_out=None):
        P.op("dve", lambda e: e.scalar_tensor_tensor(out_ap, in0, scalar, in1, op0, op1,
                                                     accum_out=accum_out), reads=reads, writes=writes)

    def cp(eng, out_ap, in_ap, reads, writes):
        if eng == "act":
            P.op("act", lambda e: e.copy(out_ap, in_ap), reads=reads, writes=writes)
        else:
            P.op(eng, lambda e: e.tensor_copy(out_ap, in_ap), reads=reads, writes=writes)

    def rstd_from_ss(rs, n, key):
        ts("dve", rs, rs, 1.0 / n, EPS, ALU.mult, ALU.add, [key], [key])
        act(rs, rs, AF.Sqrt, [key], [key])
        P.op("dve", lambda e: e.reciprocal(rs, rs), reads=[key], writes=[key])

    def psb(i):
        return PS[i][:]

    def psb16(i):
        return PS[i][:].bitcast(BF16)


    CVW = 4096
    cvreg = arena_t[:, ARENA_WORDS - CVW:ARENA_WORDS].bitcast(BF16)
    cvb = [cvreg[:, s_ * 2048:(s_ + 1) * 2048] for s_ in range(4)]
    cv_state = [0, 0]
    NCH = 128

    def conv_steps(k):
        for _ in range(k):
            c = cv_state[0]
            if c < NCH:
                sidx = c % 4
                dma("pool", cvb[sidx], peer_uv[c * 128:(c + 1) * 128, :], [], ["cv%d" % sidx], "cvl%d" % sidx)
                cv_state[0] += 1
            if cv_state[0] - cv_state[1] > 2 or (cv_state[0] == NCH and cv_state[1] < NCH):
                c2 = cv_state[1]
                sidx = c2 % 4
                dma("pool", uvb[c2 * 128:(c2 + 1) * 128, :], cvb[sidx], ["cv%d" % sidx], [], "cvs%d" % sidx)
                cv_state[1] += 1

    def conv_flush():
        while cv_state[1] < NCH:
            conv_steps(1)

    if "A" in phases:
        AR.reset()
        wtm = AR.alloc([8, TMW], BF16)
        wfm = AR.alloc([8, 3072], BF16)
        nw = AR.alloc([D])
        ident = AR.alloc([128], BF16)
        xt = [AR.alloc([D]) for _ in range(2)]
        hb = [AR.alloc([D], BF16) for _ in range(2)]
        hT = [AR.alloc([8, 128], BF16) for _ in range(2)]
        stg = [AR.alloc([512]) for _ in range(4)]
        ssq = [AR.alloc([8]) for _ in range(2)]
        dma("sp", nw, nmw, [], ["nw"], "nw")
        dma("sp", ident, c_ident, [], ["ident"], "ident")
        pieces_tm = [(0, 1792, 0), (1792, 3584, 1792), (6656, 7696, 3584), (7696, 8736, 4624)]
        pieces_fm = [(3584, 5120, 0), (5120, 6656, 1536)]
        for kc in range(8):
            for (c0, c1, d0) in pieces_tm:
                dma("pool", wtm[:, kc, d0:d0 + (c1 - c0)], w_in[kc * 128:(kc + 1) * 128, c0:c1],
                    [], ["wtm"], "wtm")
            for (c0, c1, d0) in pieces_fm:
                dma("pool", wfm[:, kc, d0:d0 + (c1 - c0)], w_in[kc * 128:(kc + 1) * 128, c0:c1],
                    [], ["wfm"], "wfm")
        bank_rr = [0]
        stg_rr = [0]
        for i in range(NT):
            b = i % 2
            kx, kh, khT, ks = "xt%d" % b, "hb%d" % b, "hT%d" % b, "ssq%d" % b
            dma("sp", xt[b], x[i * 128:(i + 1) * 128, :], [], [kx], kx)
            rs = ssq[b][:, 0:1]
            stt(hb[b], xt[b], 1.0, xt[b], ALU.mult, ALU.mult, [kx], [kh, ks], accum_out=rs)
            rstd_from_ss(rs, D, ks)
            stt(hb[b], xt[b], rs, nw, ALU.mult, ALU.mult, [kx, ks, "nw"], [kh])
            pst = "psT%d" % b
            for kc in range(8):
                tr(psb16(b)[:, kc * 128:(kc + 1) * 128], hb[b][:, kc * 128:(kc + 1) * 128], ident,
                   [kh, "ident"], [pst])
            cp("act", hT[b], psb16(b).rearrange("p (a b) -> p a b", b=128), [pst], [khT])
            ngrp = (TMW + 511) // 512
            for cg in range(ngrp):
                c0 = cg * 512
                cw = min(512, TMW - c0)
                bk = 2 + bank_rr[0] % 6
                bank_rr[0] += 1
                kb = "psb%d" % bk
                for kc in range(8):
                    mm(psb(bk)[:, 0:cw], hT[b][:, kc, :], wtm[:, kc, c0:c0 + cw], kc == 0, kc == 7,
                       [khT, "wtm"], [kb])
                s = stg_rr[0] % 4
                stg_rr[0] += 1
                ksg = "stg%d" % s
                cp("act" if cg % 2 == 0 else "dve", stg[s][:, 0:cw], psb(bk)[:, 0:cw], [kb], [ksg])
                dma("sp", pt[i * 128:(i + 1) * 128, c0:c0 + cw], stg[s][:, 0:cw], [ksg], [], ksg)
            for grp in range(6):
                bk = 2 + bank_rr[0] % 6
                bank_rr[0] += 1
                kb = "psb%d" % bk
                for j in range(4):
                    ch = grp * 4 + j
                    for kc in range(8):
                        mm(psb(bk)[:, j * 128:(j + 1) * 128], wfm[:, kc, ch * 128:(ch + 1) * 128],
                           hT[b][:, kc, :], kc == 0, kc == 7, [khT, "wfm"], [kb])
                s = stg_rr[0] % 4
                stg_rr[0] += 1
                ksg = "stg%d" % s
                cp("act" if grp % 2 == 0 else "dve", stg[s], psb(bk), [kb], [ksg])
                dst = xbcT[grp * 512:(grp + 1) * 512, i * 128:(i + 1) * 128].rearrange(
                    "(j p) t -> p j t", p=128)
                dma("sp", dst, stg[s].rearrange("p (j t) -> p j t", t=128), [ksg], [], ksg)
        P.barrier()

    if "A2" in phases:
        AR.reset()
        cw_sb = AR.alloc([24, 4])
        cb_sb = AR.alloc([24])
        u = [AR.alloc([T + 8]) for _ in range(2)]
        acc = [AR.alloc([T]) for _ in range(2)]
        dma("sp", cw_sb, convw.rearrange("p (c j) -> p c j", j=4), [], ["cw"], "cw")
        dma("sp", cb_sb, convb, [], ["cb"], "cb")
        for b in range(2):
            P.op("dve", lambda e, b=b: e.memset(u[b][:, 0:3], 0.0), writes=["u%d" % b])
        for ch in range(24):
            b = ch % 2
            ku, ka = "u%d" % b, "acc%d" % b
            dma("sp", u[b][:, 3:3 + T], xbcT[ch * 128:(ch + 1) * 128, 0:T], [], [ku], ku)
            ts("dve", acc[b], u[b][:, 0:T], cw_sb[:, ch, 0:1], None, ALU.mult, None, [ku, "cw"], [ka])
            for j in range(1, 4):
                stt(acc[b], u[b][:, j:j + T], cw_sb[:, ch, j:j + 1], acc[b], ALU.mult, ALU.add,
                    [ku, "cw", ka], [ka])
            act(acc[b], acc[b], AF.Silu, [ka, "cb"], [ka], bias=cb_sb[:, ch:ch + 1])
            dma("sp", xc[ch * 128:(ch + 1) * 128, 0:T], acc[b], [ka], [], ka)
            if "B2" in phases:
                conv_steps(2)
        assert AR.off <= ARENA_WORDS - CVW
        P.barrier()


    def run_pipelined(make_gen, n_tiles, start_every):
        active = []
        nxt = 0
        rnd = 0
        while nxt < n_tiles or active:
            if nxt < n_tiles and rnd % start_every == 0:
                active.append(make_gen(nxt))
                nxt += 1
            still = []
            for g_ in active:
                try:
                    next(g_)
                    still.append(g_)
                except StopIteration:
                    pass
            active = still
            rnd += 1


    TWO_PI = 2.0 * np.pi

    def bc(ap, shape):
        return ap.to_broadcast(list(shape))

    if "B1a" in phases:
        AR.reset()
        wao = AR.alloc([8, 1024], BF16)
        ident = AR.alloc([128], BF16)
        maskc = AR.alloc([512], BF16)
        maskp = AR.alloc([512], BF16)
        posi = AR.alloc([32], I32)
        posf = AR.alloc([32])
        invf = AR.alloc([8])
        ang = AR.alloc([32, 8])
        cs = AR.alloc([32, 8])
        sn = AR.alloc([32, 8])
        ra = AR.alloc([32, 8])
        rb_ = AR.alloc([32, 8])
        rc = AR.alloc([32, 8])
        rki = AR.alloc([32, 8], I32)
        esink = AR.alloc([16])
        qkv = [AR.alloc([1536]) for _ in range(2)]
        gt = [AR.alloc([1024]) for _ in range(2)]
        qk_bf = [AR.alloc([20, 64], BF16) for _ in range(2)]
        kd = [AR.alloc([4, 2, 128], BF16) for _ in range(2)]
        rt = [[AR.alloc([20, 8]) for _ in range(4)] for _ in range(2)]
        qT = [AR.alloc([8, 128], BF16) for _ in range(2)]
        kT = [AR.alloc([8, 128], BF16) for _ in range(3)]
        v1 = [AR.alloc([4, 66], BF16) for _ in range(3)]
        pTc = [[AR.alloc([512], BF16) for _ in range(2)] for _ in range(2)]
        pTp = [[AR.alloc([512], BF16) for _ in range(2)] for _ in range(2)]
        den = [[AR.alloc([4]) for _ in range(2)] for _ in range(2)]
        attn = [AR.alloc([16, 64], BF16) for _ in range(2)]
        attnT = [AR.alloc([8, 128], BF16) for _ in range(2)]
        gsig = [AR.alloc([1024]) for _ in range(2)]
        mres = [AR.alloc([1024]) for _ in range(2)]
        for kc in range(8):
            dma("pool", wao[:, kc, :], w_attn_o[kc * 128:(kc + 1) * 128, :], [], ["wao"], "wao")
        dma("sp", ident, c_ident, [], ["ident"], "identB")
        dma("sp", maskc, c_maskc, [], ["maskc"], "maskc")
        dma("sp", maskp, c_maskp, [], ["maskp"], "maskp")
        dma("sp", posi, pos, [], ["posi"], "posi")
        dma("sp", invf, c_invf, [], ["invf"], "invf")
        dma("sp", esink, sinks, [], ["esink"], "esink")
        act(esink, esink, AF.Exp, ["esink"], ["esink"])
        cp("dve", posf, posi, ["posi"], ["posf"])
        tt("dve", ang, bc(posf.unsqueeze(2), [128, 32, 8]), bc(invf.unsqueeze(1), [128, 32, 8]), ALU.mult,
           ["posf", "invf"], ["ang"])

        def sin_of(dst, dkey, shift):
            ts("dve", ra, ang, shift, 1.0 / TWO_PI, ALU.add, ALU.mult, ["ang"], ["ra"])
            cp("dve", rki, ra, ["ra"], ["rki"])
            cp("dve", rb_, rki, ["rki"], ["rb"])
            ts("dve", ra, ang, shift, None, ALU.add, None, ["ang"], ["ra"])
            stt(ra, rb_, -TWO_PI, ra, ALU.mult, ALU.add, ["rb", "ra"], ["ra"])
            ts("dve", rb_, ra, float(np.pi), None, ALU.is_gt, None, ["ra"], ["rb"])
            ts("dve", rc, ra, float(-np.pi), None, ALU.is_lt, None, ["ra"], ["rc"])
            tt("dve", rc, rc, rb_, ALU.subtract, ["rb", "rc"], ["rc"])
            stt(ra, rc, TWO_PI, ra, ALU.mult, ALU.add, ["rc", "ra"], ["ra"])
            ts("dve", ra, ra, float(np.pi), float(-np.pi), ALU.min, ALU.max, ["ra"], ["ra"])
            act(dst, ra, AF.Sin, ["ra"], [dkey])

        sin_of(sn, "sn", 0.0)
        sin_of(cs, "cs", float(np.pi / 2))
        for b in range(3):
            P.op("dve", lambda e, b=b: e.memset(v1[b][:, :, 64:66], 1.0), writes=["v1_%d" % b])
        for b in range(2):
            P.op("pool", lambda e, b=b: e.memset(kd[b], 0.0), writes=["kd%d" % b])

        def b1a_tile(i):
            b = i % 2
            b3 = i % 3
            pb3 = (i - 1) % 3
            S = lambda n: "%s_%d" % (n, b)
            kq, kg = "qkv%d" % b, "gt%d" % b
            rows = slice(i * 128, (i + 1) * 128)
            dma("sp", qkv[b], pt[rows, 0:1536], [], [kq], kq)
            dma("sp", gt[b], pt[rows, 3616:4640], [], [kg], kg)
            qk = qkv[b][:, 0:1280].rearrange("p (h d) -> p h d", d=64)
            x1 = qk[:, :, 0:8]
            x2 = qk[:, :, 8:16]
            cb_ = bc(cs[:, i:i + 1, :], [128, 20, 8])
            sb_ = bc(sn[:, i:i + 1, :], [128, 20, 8])
            r_ = rt[b]
            qb = qk_bf[b]
            tt("dve", r_[0], x1, cb_, ALU.mult, [kq, "cs"], [S("rt0")])
            tt("dve", r_[1], x2, sb_, ALU.mult, [kq, "sn"], [S("rt1")])
            tt("dve", qb[:, :, 0:8], r_[0], r_[1], ALU.subtract, [S("rt0"), S("rt1")], [S("qkbf")])
            tt("dve", r_[2], x2, cb_, ALU.mult, [kq, "cs"], [S("rt2")])
            tt("dve", r_[3], x1, sb_, ALU.mult, [kq, "sn"], [S("rt3")])
            tt("dve", qb[:, :, 8:16], r_[2], r_[3], ALU.add, [S("rt2"), S("rt3")], [S("qkbf")])
            cp("pool", qb[:, :, 16:64], qk[:, :, 16:64], [kq], [S("qkbf2")])
            cp("pool", kd[b][:, :, 0, 0:64], qb[:, 16:20, :], [S("qkbf"), S("qkbf2")], ["kd%d" % b])
            cp("pool", kd[b][:, :, 1, 64:128], qb[:, 16:20, :], [S("qkbf"), S("qkbf2")], ["kd%d" % b])
            cp("act", v1[b3][:, :, 0:64], qkv[b][:, 1280:1536].rearrange("p (h d) -> p h d", d=64),
               [kq], ["v1_%d" % b3])
            kpsT = "ps%d" % b
            for j in range(8):
                tr(psb16(b)[:, j * 128:(j + 1) * 128],
                   qb[:, 2 * j:2 * j + 2, :].rearrange("p a b -> p (a b)"), ident,
                   [S("qkbf"), S("qkbf2"), "ident"], [kpsT])
            cp("act", qT[b], psb16(b).rearrange("p (a b) -> p a b", b=128), [kpsT], [S("qT")])
            for h2 in range(8):
                tr(psb16(b)[:, h2 * 128:(h2 + 1) * 128], kd[b][:, h2 // 2, h2 % 2, :], ident,
                   ["kd%d" % b, "ident"], [kpsT])
            cp("dve", kT[b3], psb16(b).rearrange("p (a b) -> p a b", b=128), [kpsT], ["kT%d" % b3])
            yield
            for h in range(4):
                hp = h % 2
                bc_, bp_, bo_ = 2 + hp * 2, 3 + hp * 2, 6 + hp
                kbc, kbp, kbo = "ps%d" % bc_, "ps%d" % bp_, "ps%d" % bo_
                pc, pp = pTc[b][hp], pTp[b][hp]
                kpc, kpp = "pTc%d_%d" % (b, hp), "pTp%d_%d" % (b, hp)
                blocks = [(bc_, kbc, maskc, "maskc", kT[b3], "kT%d" % b3, pc, kpc)]
                if i > 0:
                    blocks.append((bp_, kbp, maskp, "maskp", kT[pb3], "kT%d" % pb3, pp, kpp))
                for (bk, kb, msk, kmsk, kTt, kkT, pT_, kpT) in blocks:
                    for g in range(4):
                        half = g % 2
                        mm(psb(bk)[:, g * 128:(g + 1) * 128], ident, msk[:, 0:128], True, False,
                           ["ident", kmsk], [kb])
                        mm(psb(bk)[:, g * 128:(g + 1) * 128], kTt[:, 2 * h + half, :],
                           qT[b][:, 2 * h + g // 2, :], False, True,
                           [kkT, S("qT")], [kb])
                    act(pT_, psb(bk), AF.Exp, [kb], [kpT], scale=0.125)
                for g in range(4):
                    o_ap = psb(bo_)[:, g * 65:(g + 1) * 65]
                    if i > 0:
                        mm(o_ap, pp[:, g * 128:(g + 1) * 128], v1[pb3][:, h, 0:65], True, False,
                           [kpp, "v1_%d" % pb3], [kbo])
                        mm(o_ap, pc[:, g * 128:(g + 1) * 128], v1[b3][:, h, 0:65], False, True,
                           [kpc, "v1_%d" % b3], [kbo])
                    else:
                        mm(o_ap, pc[:, g * 128:(g + 1) * 128], v1[b3][:, h, 0:65], True, True,
                           [kpc, "v1_%d" % b3], [kbo])
                ov = psb(bo_)[:, 0:260].rearrange("p (g d) -> p g d", d=65)
                dn = den[b][hp]
                kden = "den%d_%d" % (b, hp)
                tt("dve", dn, ov[:, :, 64], esink[:, 4 * h:4 * h + 4], ALU.add, [kbo, "esink"], [kden])
                P.op("dve", lambda e, dn=dn: e.reciprocal(dn, dn), reads=[kden], writes=[kden])
                tt("dve", attn[b][:, 4 * h:4 * h + 4, :], ov[:, :, 0:64], bc(dn.unsqueeze(2), [128, 4, 64]),
                   ALU.mult, [kbo, kden], [S("attn")])
                yield
            for j in range(8):
                tr(psb16(b)[:, j * 128:(j + 1) * 128],
                   attn[b][:, 2 * j:2 * j + 2, :].rearrange("p a b -> p (a b)"), ident, [S("attn"), "ident"], [kpsT])
            cp("act", attnT[b], psb16(b).rearrange("p (a b) -> p a b", b=128), [kpsT], [S("attnT")])
            act(gsig[b], gt[b], AF.Sigmoid, [kg], [S("gsig")])
            for cg in range(2):
                bk = 2 + cg + 2 * b
                for kc in range(8):
                    mm(psb(bk), attnT[b][:, kc, :], wao[:, kc, cg * 512:(cg + 1) * 512], kc == 0, kc == 7,
                       [S("attnT"), "wao"], ["ps%d" % bk])
                tt("dve", mres[b][:, cg * 512:(cg + 1) * 512], psb(bk), gsig[b][:, cg * 512:(cg + 1) * 512],
                   ALU.mult, ["ps%d" % bk, S("gsig")], ["mres%d" % b])
            dma("sp", ma[rows, :], mres[b], ["mres%d" % b], [], "mres%d" % b)
            if "B2" in phases:
                conv_steps(3)
            yield

        run_pipelined(b1a_tile, NT, 3)
        if "B2" in phases:
            conv_flush()
        assert AR.off <= ARENA_WORDS - CVW
        P.barrier()

    if "B1b" in phases:
        AR.reset()
        wso = AR.alloc([16, 1024], BF16)
        wo = AR.alloc([8, 1024], BF16)
        ident = AR.alloc([128], BF16)
        identf = AR.alloc([128])
        maskc = AR.alloc([512], BF16)
        Um = AR.alloc([128])
        Lm = AR.alloc([128])
        onesm = AR.alloc([128])
        snw_sb = AR.alloc([16])
        dtb_sb = AR.alloc([32])
        A_sb = AR.alloc([32])
        dsk_sb = AR.alloc([32])
        one_c = AR.alloc([8])
        stateT = AR.alloc([4, 512])
        stateT_bf = AR.alloc([4, 512], BF16)
        zt = [AR.alloc([2048]) for _ in range(2)]
        dtr = [AR.alloc([32]) for _ in range(2)]
        g2 = [AR.alloc([1024]) for _ in range(2)]
        xct = [AR.alloc([24, 128]) for _ in range(2)]
        mat = [AR.alloc([1024]) for _ in range(2)]
        xt = [AR.alloc([1024]) for _ in range(2)]
        dtt = [AR.alloc([32]) for _ in range(2)]
        dA = [AR.alloc([32]) for _ in range(2)]
        acum_sb = [AR.alloc([32]) for _ in range(2)]
        ea = [AR.alloc([32]) for _ in range(2)]
        wd = [AR.alloc([32]) for _ in range(2)]
        etot = [AR.alloc([32]) for _ in range(2)]
        wdt = [AR.alloc([32]) for _ in range(2)]
        BCbf = [AR.alloc([8, 128], BF16) for _ in range(2)]
        B_tm = [AR.alloc([4, 128], BF16) for _ in range(2)]
        ynT = [AR.alloc([16, 128], BF16) for _ in range(2)]
        xdt = AR.alloc([8, 64], BF16)
        xdtw = AR.alloc([8, 64], BF16)
        xsD = AR.alloc([8, 64], BF16)
        cbT = AR.alloc([128])
        Rm = [AR.alloc([4, 128]) for _ in range(2)]
        decT = [AR.alloc([4, 128]) for _ in range(2)]
        MT = [AR.alloc([4, 128], BF16) for _ in range(2)]
        ytmp = AR.alloc([8, 64])
        yg = AR.alloc([8, 64])
        ssg = AR.alloc([8])
        yn_g = AR.alloc([512], BF16)
        mtmp = AR.alloc([1024])
        mbf = AR.alloc([1024], BF16)
        mT = AR.alloc([8, 128], BF16)
        if debug:
            print("B1b arena words", AR.off)
        dma("sp", snw_sb, snwc, [], ["snw"], "snw")
        for kc in range(16):
            sb = kc % 2
            stg_ = zt[sb][:, 0:1024]
            dma("sp", stg_, w_ssd_o[kc * 128:(kc + 1) * 128, :], [], ["zt%d" % sb], "zt%d" % sb)
            ts("dve", wso[:, kc, :], stg_, snw_sb[:, kc:kc + 1], None, ALU.mult, None,
               ["zt%d" % sb, "snw"], ["wso"])
        for kc in range(8):
            dma("pool", wo[:, kc, :], w_out[kc * 128:(kc + 1) * 128, :], [], ["wo"], "wo")
        dma("sp", ident, c_ident, [], ["ident"], "identC")
        dma("sp", identf, c_identf, [], ["identf"], "identf")
        dma("sp", maskc, c_maskc, [], ["maskc"], "maskcC")
        dma("sp", Um, c_U, [], ["Um"], "Um")
        dma("sp", Lm, c_L, [], ["Lm"], "Lm")
        dma("sp", onesm, c_ones, [], ["onesm"], "onesm")
        dma("sp", dtb_sb, dtb, [], ["dtb"], "dtb")
        dma("sp", A_sb, alog, [], ["A"], "A")
        dma("sp", dsk_sb, dskip, [], ["dsk"], "dsk")
        act(A_sb, A_sb, AF.Exp, ["A"], ["A"])
        ts("dve", A_sb, A_sb, -1.0, None, ALU.mult, None, ["A"], ["A"])
        P.op("dve", lambda e: e.memset(one_c, 1.0), writes=["one_c"])
        P.op("dve", lambda e: e.memset(stateT, 0.0), writes=["stateT"])
        P.op("dve", lambda e: e.memset(stateT_bf, 0.0), writes=["stateTbf"])
        rrb = [0]

        def nbank():
            bk = rrb[0] % 8
            rrb[0] += 1
            return bk, "ps%d" % bk

        def b1b_tile(i):
            b = i % 2
            S = lambda n: "%s_%d" % (n, b)
            rows = slice(i * 128, (i + 1) * 128)
            kzt, kdtr, kg2, kxct, kmat, kxt = ("zt%d" % b, "dtr%d" % b, "g2_%d" % b, "xct%d" % b,
                                               "mat%d" % b, "xtB%d" % b)
            dma("sp", dtr[b], pt[rows, 3584:3616], [], [kdtr], kdtr)
            dma("sp", xct[b], xc[:, i * 128:(i + 1) * 128].rearrange("(c p) t -> p c t", p=128), [], [kxct], kxct)
            dma("sp", zt[b], pt[rows, 1536:3584], [], [kzt], kzt)
            dma("sp", g2[b], pt[rows, 4640:5664], [], [kg2], kg2)
            dma("sp", mat[b], ma[rows, :], [], [kmat], kmat)
            dma("sp", xt[b], x[rows, :], [], [kxt], kxt)
            tt("dve", dtt[b], dtr[b], dtb_sb, ALU.add, [kdtr, "dtb"], [S("dtt")])
            act(dtt[b], dtt[b], AF.Exp, [S("dtt")], [S("dtt")])
            act(dtt[b], dtt[b], AF.Ln, [S("dtt"), "one_c"], [S("dtt")], bias=one_c[:, 0:1])
            tt("dve", dA[b], dtt[b], A_sb, ALU.mult, [S("dtt"), "A"], [S("dA")])
            bk, kb = nbank()
            mm(psb(bk)[:, 0:32], Um, dA[b], True, True, ["Um", S("dA")], [kb])
            mm(psb(bk)[:, 32:64], onesm, dA[b], True, True, ["onesm", S("dA")], [kb])
            cp("dve", acum_sb[b], psb(bk)[:, 0:32], [kb], [S("acum")])
            act(ea[b], psb(bk)[:, 0:32], AF.Exp, [kb], [S("ea")])
            act(etot[b], psb(bk)[:, 32:64], AF.Exp, [kb], [S("etot")])
            tt("dve", wd[b], psb(bk)[:, 32:64], acum_sb[b], ALU.subtract, [kb, S("acum")], [S("wd")])
            act(wd[b], wd[b], AF.Exp, [S("wd")], [S("wd")])
            tt("dve", wdt[b], wd[b], dtt[b], ALU.mult, [S("wd"), S("dtt")], [S("wdt")])
            cp("act", BCbf[b], xct[b][:, 16:24, :], [kxct], [S("BCbf")])
            bk, kb = nbank()
            for g in range(4):
                tr(psb16(bk)[:, g * 128:(g + 1) * 128], BCbf[b][:, g, :], ident, [S("BCbf"), "ident"], [kb])
            cp("act", B_tm[b], psb16(bk)[:, 0:512].rearrange("p (a b) -> p a b", b=128), [kb], [S("B_tm")])
            yield
            for g in range(4):
                hs = slice(8 * g, 8 * g + 8)
                bkx, kx = nbank()
                for j in range(4):
                    tr(psb(bkx)[:, j * 128:(j + 1) * 128], xct[b][:, 4 * g + j, :], identf, [kxct, "identf"], [kx])
                xv = psb(bkx).rearrange("p (h d) -> p h d", d=64)
                tt("dve", xdt, xv, bc(dtt[b][:, hs].unsqueeze(2), [128, 8, 64]), ALU.mult, [kx, S("dtt")], ["xdt"])
                tt("dve", xdtw, xv, bc(wdt[b][:, hs].unsqueeze(2), [128, 8, 64]), ALU.mult, [kx, S("wdt")], ["xdtw"])
                tt("dve", xsD, xv, bc(dsk_sb[:, hs].unsqueeze(2), [128, 8, 64]), ALU.mult, [kx, "dsk"], ["xsD"])
                bk, kb = nbank()
                mm(psb(bk)[:, 0:128], BCbf[b][:, g, :], BCbf[b][:, 4 + g, :], True, True, [S("BCbf")], [kb])
                cp("act", cbT, psb(bk)[:, 0:128], [kb], ["cbT"])
                bky, kby = nbank()
                for blk in range(2):
                    h0 = 8 * g + 4 * blk
                    rb2 = blk
                    tt("pool", Rm[rb2], bc(Um.unsqueeze(1), [128, 4, 128]),
                       bc(dA[b][:, h0:h0 + 4].unsqueeze(2), [128, 4, 128]), ALU.mult, ["Um", S("dA")], ["Rm%d" % rb2])
                    bkd, kbd = nbank()
                    mm(psb(bkd), ident, maskc, True, False, ["ident", "maskc"], [kbd])
                    mm(psb(bkd), Lm, Rm[rb2].rearrange("p a b -> p (a b)"), False, True, ["Lm", "Rm%d" % rb2], [kbd])
                    act(decT[rb2], psb(bkd).rearrange("p (a b) -> p a b", b=128), AF.Exp, [kbd], ["decT%d" % rb2])
                    tt("dve", MT[rb2], decT[rb2], bc(cbT.unsqueeze(1), [128, 4, 128]), ALU.mult,
                       ["decT%d" % rb2, "cbT"], ["MT%d" % rb2])
                    for j in range(4):
                        hh = 4 * blk + j
                        mm(psb(bky)[:, hh * 64:(hh + 1) * 64], ident, xsD[:, hh, :], True, False,
                           ["ident", "xsD"], [kby])
                        mm(psb(bky)[:, hh * 64:(hh + 1) * 64], MT[rb2][:, j, :], xdt[:, hh, :], False, True,
                           ["MT%d" % rb2, "xdt"], [kby])
                bki, kbi = nbank()
                mm(psb(bki), BCbf[b][:, 4 + g, :], stateT_bf[:, g, :], True, True, [S("BCbf"), "stateTbf"], [kbi])
                tt("dve", ytmp, psb(bki).rearrange("p (h d) -> p h d", d=64),
                   bc(ea[b][:, hs].unsqueeze(2), [128, 8, 64]), ALU.mult, [kbi, S("ea")], ["ytmp"])
                tt("dve", yg, psb(bky).rearrange("p (h d) -> p h d", d=64), ytmp, ALU.add, [kby, "ytmp"], ["yg"])
                bks, kbs = nbank()
                mm(psb(bks), B_tm[b][:, g, :], xdtw.rearrange("p a b -> p (a b)"), True, True,
                   [S("B_tm"), "xdtw"], [kbs])
                sv = stateT[:, g, :].rearrange("p (h d) -> p h d", d=64)
                tt("pool", sv, sv, bc(etot[b][:, hs].unsqueeze(2), [128, 8, 64]), ALU.mult, ["stateT", S("etot")], ["stateT"])
                tt("dve", stateT[:, g, :], stateT[:, g, :], psb(bks), ALU.add, ["stateT", kbs], ["stateT"])
                cp("act", stateT_bf[:, g, :], stateT[:, g, :], ["stateT"], ["stateTbf"])
                zg = zt[b][:, g * 512:(g + 1) * 512]
                act(zg, zg, AF.Silu, [kzt], [kzt])
                ygf = yg.rearrange("p a b -> p (a b)")
                tt("dve", ygf, ygf, zg, ALU.mult, ["yg", kzt], ["yg"])
                rs = ssg[:, g:g + 1]
                stt(ytmp.rearrange("p a b -> p (a b)"), ygf, 1.0, ygf, ALU.mult, ALU.mult, ["yg"], ["ytmp", "ssg"], accum_out=rs)
                rstd_from_ss(rs, 512, "ssg")
                ts("dve", yn_g, ygf, rs, None, ALU.mult, None, ["yg", "ssg"], ["yn_g"])
                bk, kb = nbank()
                for j in range(4):
                    tr(psb16(bk)[:, j * 128:(j + 1) * 128], yn_g[:, j * 128:(j + 1) * 128], ident, ["yn_g", "ident"], [kb])
                cp("act", ynT[b][:, 4 * g:4 * g + 4, :], psb16(bk)[:, 0:512].rearrange("p (a b) -> p a b", b=128), [kb], [S("ynT")])
                yield
            act(g2[b], g2[b], AF.Sigmoid, [kg2], [kg2])
            for cg in range(2):
                bk, kb = nbank()
                cs_ = slice(cg * 512, (cg + 1) * 512)
                for kc in range(16):
                    mm(psb(bk), ynT[b][:, kc, :], wso[:, kc, cs_], kc == 0, kc == 15, [S("ynT"), "wso"], [kb])
                tt("dve", mtmp[:, cs_], psb(bk), g2[b][:, cs_], ALU.mult, [kb, kg2], ["mtmp"])
                tt("dve", mbf[:, cs_], mtmp[:, cs_], mat[b][:, cs_], ALU.add, ["mtmp", kmat], ["mbf"])
            bk, kb = nbank()
            for j in range(8):
                tr(psb16(bk)[:, j * 128:(j + 1) * 128], mbf[:, j * 128:(j + 1) * 128], ident, ["mbf", "ident"], [kb])
            cp("act", mT, psb16(bk).rearrange("p (a b) -> p a b", b=128), [kb], ["mT"])
            for cg in range(2):
                bk, kb = nbank()
                cs_ = slice(cg * 512, (cg + 1) * 512)
                for kc in range(8):
                    mm(psb(bk), mT[:, kc, :], wo[:, kc, cs_], kc == 0, kc == 7, ["mT", "wo"], [kb])
                tt("dve", mtmp[:, cs_], psb(bk), xt[b][:, cs_], ALU.add, [kb, kxt], ["mtmp"])
            dma("sp", x1s[rows, :], mtmp, ["mtmp"], [], "mtmp")
            yield

        run_pipelined(b1b_tile, NT, 3)
        P.barrier()

    if "B2" in phases:
        if "A2" not in phases or "B1a" not in phases:
            conv_flush()
            P.barrier()
        AR.reset()
        NB = 14
        GS = 4
        wq = AR.alloc([8, 2048], BF16)
        kTs = AR.alloc([16, 128], BF16)
        ident = AR.alloc([128], BF16)
        identf = AR.alloc([128])
        nfw_sb = AR.alloc([1024])
        nlw_sb = AR.alloc([1024])
        iota_sb = AR.alloc([128, 16], BF16)
        x1t = [AR.alloc([1024]) for _ in range(2)]
        hx = AR.alloc([1024])
        hbf = [AR.alloc([1024], BF16) for _ in range(2)]
        prod = [AR.alloc([1024], BF16) for _ in range(4)]
        junkA = AR.alloc([1024], BF16)
        hnT = AR.alloc([8, 128], BF16)
        qpT = AR.alloc([16, 128], BF16)
        s_all = AR.alloc([16, 128])
        work = AR.alloc([2048])
        s_w = work.rearrange("p (a b) -> p a b", b=128)
        cand_w = work.rearrange("p (a b) -> p a b", b=256)
        oh = work.rearrange("p (a b) -> p a b", b=16)
        vals = AR.alloc([16, 16])
        idxu = AR.alloc([16, 16], U32)
        i12f = AR.alloc([16, 16])
        cand = s_all.rearrange("p a b -> p (a b)").rearrange("p (a b) -> p a b", b=256)
        sc = AR.alloc([8, 16])
        flat = AR.alloc([8, 16], U32)
        fab = AR.alloc([8, 16], U32)
        fabf = AR.alloc([8, 16])
        e1f = AR.alloc([128])
        e2f = AR.alloc([128])
        idxf = AR.alloc([128])
        idx_i = [AR.alloc([128], I32) for _ in range(2)]
        es = AR.alloc([8, 16])
        ssum = AR.alloc([8])
        gsm = [AR.alloc([128]) for _ in range(2)]
        a_all = [AR.alloc([128]) for _ in range(2)]
        w_all = AR.alloc([128])
        w2 = AR.alloc([128])
        rs2 = AR.alloc([8])
        rs3 = AR.alloc([8])
        junk = AR.alloc([1024], BF16)
        junk2 = AR.alloc([1024])
        accv = AR.alloc([1024])
        dg4 = [AR.alloc([GS, 128], BF16) for _ in range(2)]
        gbuf = [AR.alloc([2048], BF16) for _ in range(NB)]
        for kc in range(8):
            dma("pool", wq[:, kc, :], peer_wq[kc * 128:(kc + 1) * 128, :], [], ["wq"], "wq")
        dma("pool", kTs, keysT.rearrange("p (a b) -> p a b", b=128), [], ["kTs"], "kTs")
        dma("sp", ident, c_ident, [], ["ident"], "identD")
        dma("sp", identf, c_identf, [], ["identf"], "identfD")
        dma("sp", nfw_sb, nfw, [], ["nfw"], "nfw")
        dma("sp", nlw_sb, nlw, [], ["nlw"], "nlw")
        dma("sp", iota_sb, c_iota.rearrange("p (a b) -> p a b", b=16), [], ["iota"], "iota")
        gcnt = [0]
        dcnt = [0]
        rrb2 = [0]
        if debug:
            print('B2 arena words', AR.off)

        def nbank2():
            bk = rrb2[0] % 4
            rrb2[0] += 1
            return bk, "ps%d" % bk

        def top16(src, wk, vout, iout, ksrc, kwork, kv, ki):
            P.op("dve", lambda e: e.max(out=vout[:, 0:8], in_=src), reads=[ksrc], writes=[kv])
            P.op("dve", lambda e: e.max_index(out=iout[:, 0:8], in_max=vout[:, 0:8], in_values=src),
                 reads=[ksrc, kv], writes=[ki])
            P.op("dve", lambda e: e.match_replace(out=wk, in_to_replace=vout[:, 0:8], in_values=src,
                                                  imm_value=-1e30), reads=[ksrc, kv], writes=[kwork])
            P.op("dve", lambda e: e.max(out=vout[:, 8:16], in_=wk), reads=[kwork], writes=[kv])
            P.op("dve", lambda e: e.max_index(out=iout[:, 8:16], in_max=vout[:, 8:16], in_values=wk),
                 reads=[kwork, kv], writes=[ki])

        def front(i):
            b = i % 2
            rows = slice(i * 128, (i + 1) * 128)
            kx, khx = "x1t%d" % b, "hx%d" % b
            dma("sp", x1t[b], x1s[rows, :], [], [kx], kx)
            rs = rs2[:, 0:1]
            stt(hx, x1t[b], 1.0, x1t[b], ALU.mult, ALU.mult, [kx], ["hxf", "rs2"], accum_out=rs)
            yield
            rstd_from_ss(rs, D, "rs2")
            yield
            stt(hbf[b], x1t[b], rs, nfw_sb, ALU.mult, ALU.mult, [kx, "rs2", "nfw"], [khx])
            yield
            bk, kb = nbank2()
            for kc in range(8):
                tr(psb16(bk)[:, kc * 128:(kc + 1) * 128], hbf[b][:, kc * 128:(kc + 1) * 128], ident, [khx, "ident"], [kb])
            cp("act", hnT, psb16(bk).rearrange("p (a b) -> p a b", b=128), [kb], ["hnT"])
            for grp in range(4):
                bk, kb = nbank2()
                for j in range(4):
                    ch = 4 * grp + j
                    for kc in range(8):
                        mm(psb(bk)[:, j * 128:(j + 1) * 128], wq[:, kc, ch * 128:(ch + 1) * 128], hnT[:, kc, :],
                           kc == 0, kc == 7, ["wq", "hnT"], [kb])
                cp("act", qpT[:, 4 * grp:4 * grp + 4, :], psb(bk).rearrange("p (a b) -> p a b", b=128), [kb], ["qpT"])
            for grp in range(4):
                bk, kb = nbank2()
                for j in range(4):
                    hc = 4 * grp + j
                    mm(psb(bk)[:, j * 128:(j + 1) * 128], qpT[:, hc, :], kTs[:, hc, :], True, True, ["qpT", "kTs"], [kb])
                cp("act", s_all[:, 4 * grp:4 * grp + 4, :], psb(bk).rearrange("p (a b) -> p a b", b=128), [kb], ["scand"])
            for _sp in range(8):
                yield
            for hc in range(16):
                top16(s_all[:, hc, :], s_w[:, hc, :], vals[:, hc, :], idxu[:, hc, :], "scand", "work", "vals", "idxu")
                yield
            cp("act", i12f, idxu, ["idxu"], ["i12f"])
            vals4 = vals.rearrange("p (h c) k -> p h c k", c=2)
            i12f4 = i12f.rearrange("p (h c) k -> p h c k", c=2)
            tt("dve", cand.rearrange("p h (a b) -> p h a b", b=16),
               bc(vals4[:, :, 0, :].unsqueeze(3), [128, 8, 16, 16]),
               bc(vals4[:, :, 1, :].unsqueeze(2), [128, 8, 16, 16]), ALU.add, ["vals"], ["scand"])
            yield
            for h in range(8):
                top16(cand[:, h, :], cand_w[:, h, :], sc[:, h, :], flat[:, h, :], "scand", "work", "sc", "flat")
                yield
            for which, (dstf, sh_op, sh_val) in enumerate([(e1f, ALU.logical_shift_right, 4), (e2f, ALU.bitwise_and, 15)]):
                ts("dve", fab, flat, sh_val, None, sh_op, None, ["flat"], ["fab"])
                cp("act", fabf, fab, ["fab"], ["fabf"])
                yield
                yield
                tt("dve", oh, iota_sb, bc(fabf.rearrange("p a b -> p (a b)").unsqueeze(2), [128, 128, 16]),
                   ALU.is_equal, ["iota", "fabf"], ["work"])
                yield
                oh4 = oh.rearrange("p (h k) a -> p h k a", k=16)
                tt("dve", oh4, oh4, bc(i12f4[:, :, which, :].unsqueeze(2), [128, 8, 16, 16]), ALU.mult,
                   ["work", "i12f"], ["work"])
                yield
                P.op("dve", lambda e, dstf=dstf: e.tensor_reduce(out=dstf, in_=oh, axis=AX.X, op=ALU.add),
                     reads=["work"], writes=["e12_%d" % which])
                yield
            stt(idxf, e1f, 128.0, e2f, ALU.mult, ALU.add, ["e12_0", "e12_1"], ["idxf"])
            cp("dve", idx_i[b], idxf, ["idxf"], ["idx%d" % b])
            tt("pool", es, sc, bc(sc[:, :, 0:1], [128, 8, 16]), ALU.subtract, ["sc"], ["es"])
            act(es, es, AF.Exp, ["es"], ["es"])
            yield
            yield
            P.op("dve", lambda e: e.tensor_reduce(out=ssum, in_=es, axis=AX.X, op=ALU.add), reads=["es"], writes=["ssum"])
            P.op("dve", lambda e: e.reciprocal(ssum, ssum), reads=["ssum"], writes=["ssum"])
            tt("pool", gsm[b].rearrange("p (a b) -> p a b", b=16), es, bc(ssum.unsqueeze(2), [128, 8, 16]), ALU.mult,
               ["es", "ssum"], ["gsm%d" % b])
            yield

        def back(i, tail_prev):
            b = i % 2
            kx, khx, kidx = "x1t%d" % b, "hx%d" % b, "idx%d" % b
            pa = 4 + 2 * b
            ngrp = 128 // GS
            pendq = []

            def finish(j0, bufs):
                js = slice(j0, j0 + GS)
                dk = dcnt[0] % 2
                dcnt[0] += 1
                tt("pool", w2[:, js], w_all[:, js], gsm[b][:, js], ALU.mult, ["w_all", "gsm%d" % b], ["w2"])
                tt("pool", dg4[dk], bc(ident.unsqueeze(1), [128, GS, 128]),
                   bc(w2[:, js].unsqueeze(2), [128, GS, 128]), ALU.mult, ["ident", "w2"], ["dg%d" % dk])
                for jj, j in enumerate(range(j0, j0 + GS)):
                    bf, kgb = bufs[jj]
                    for half in range(2):
                        mm(psb(pa + half), dg4[dk][:, jj, :], gbuf[bf][:, 1024 + half * 512:1024 + (half + 1) * 512],
                           False, j == 127, ["dg%d" % dk, kgb], ["ps%d" % (pa + half)])

            for half in range(2):
                mm(psb(pa + half), identf, x1t[b][:, half * 512:(half + 1) * 512], True, False,
                   ["identf", kx], ["ps%d" % (pa + half)])
            for g_ in range(ngrp):
                j0 = g_ * GS
                bufs = []
                for j in range(j0, j0 + GS):
                    bf = gcnt[0] % NB
                    gcnt[0] += 1
                    kgb = "g%d" % bf
                    bufs.append((bf, kgb))
                    P.op("pool", lambda e, bf=bf, j=j, b=b: e.indirect_dma_start(
                        out=gbuf[bf], out_offset=None, in_=uvb,
                        in_offset=bass.IndirectOffsetOnAxis(ap=idx_i[b][:, j:j + 1], axis=0)),
                        reads=[kidx], writes=[kgb], dma=True, semkey=kgb)
                    pk = pcnt[0] % 4
                    pcnt[0] += 1
                    tt("dve", prod[pk], gbuf[bf][:, 0:1024], hbf[b], ALU.mult, [kgb, khx], ["prod%d" % pk])
                    act(junkA, prod[pk], AF.Copy, ["prod%d" % pk], ["junkA", "a_all"],
                        accum_out=a_all[b][:, j:j + 1])
                js = slice(j0, j0 + GS)
                act(w_all[:, js], a_all[b][:, js], AF.Gelu, ["a_all"], ["w_all"])
                pendq.append((j0, bufs))
                if len(pendq) > 2:
                    finish(*pendq.pop(0))
                if g_ == 2 and tail_prev is not None:
                    tail_prev()
                yield
            while pendq:
                finish(*pendq.pop(0))

        def make_tail(i):
            b = i % 2
            rows = slice(i * 128, (i + 1) * 128)
            kx = "x1t%d" % b
            pa = 4 + 2 * b

            def tail():
                for half in range(2):
                    hs_ = slice(half * 512, (half + 1) * 512)
                    cp("act", accv[:, hs_], psb(pa + half), ["ps%d" % (pa + half)], ["accv"])
                rs = rs3[:, 0:1]
                stt(junk2, accv, 1.0, accv, ALU.mult, ALU.mult, ["accv"], ["junk2", "rs3"], accum_out=rs)
                rstd_from_ss(rs, D, "rs3")
                stt(junk2, accv, rs, nlw_sb, ALU.mult, ALU.mult, ["accv", "rs3", "nlw"], ["junk2"])
                dma("sp", out[rows, :], junk2, ["junk2"], [], "junk2")
            return tail

        pcnt = [0]
        for _ in front(0):
            pass
        tail_prev = None
        for i in range(NT):
            gf = front(i + 1) if i + 1 < NT else None
            for _ in back(i, tail_prev):
                if gf is not None:
                    for _k in range(3):
                        try:
                            next(gf)
                        except StopIteration:
                            gf = None
                            break
            if gf is not None:
                for _ in gf:
                    pass
            tail_prev = make_tail(i)
        tail_prev()
        P.barrier()

    outkeys = [k for k in P.dma_cnt]
    P.emit(out_semkeys=outkeys)
    st.close()
    return nc


def host_consts():
    bf = ml_dtypes.bfloat16
    k = np.arange(128)[:, None]
    q = np.arange(128)[None, :]
    mc = np.where(k <= q, 0.0, NEG).astype(np.float32)
    mp = np.where(k > q, 0.0, NEG).astype(np.float32)
    c = {
        "c_ident": np.eye(128, dtype=np.float32).astype(bf),
        "c_identf": np.eye(128, dtype=np.float32),
        "c_maskc": np.tile(mc, (1, 4)).astype(bf),
        "c_maskp": np.tile(mp, (1, 4)).astype(bf),
        "c_U": (k <= q).astype(np.float32),
        "c_L": (k > q).astype(np.float32),
        "c_ones": np.ones((128, 128), np.float32),
        "c_iota": np.tile(np.arange(16, dtype=np.float32), (128, 128)).astype(bf),
        "c_invf": np.tile((500000.0 ** (-np.arange(0, 16, 2, dtype=np.float32) / 16.0)).astype(np.float32),
                          (128, 1)),
    }
    return c


def rep(v, n=128):
    return np.ascontiguousarray(np.broadcast_to(np.asarray(v, np.float32).reshape(1, -1), (n, v.size)))


def make_in_maps(inputs, ncores=8):
    g = lambda k: np.asarray(inputs[k])
    shared = dict(host_consts())
    shared["w_in"] = np.ascontiguousarray(g("w_in")[0])
    shared["w_attn_o"] = np.ascontiguousarray(g("w_attn_o")[0])
    shared["w_ssd_o"] = np.ascontiguousarray(g("w_ssd_o")[0])
    shared["w_out"] = np.ascontiguousarray(g("w_out")[0])
    shared["peer_wq"] = np.ascontiguousarray(g("peer_wq")[0])
    shared["keysT"] = np.ascontiguousarray(g("peer_keys")[0].transpose(3, 0, 1, 2).reshape(128, 2048))
    shared["peer_uv"] = np.ascontiguousarray(
        np.concatenate([g("peer_u")[0], g("peer_v")[0]], axis=1))
    shared["nmw"] = rep(g("norm_mix_w")[0])
    shared["nfw"] = rep(g("norm_ffn_w")[0])
    shared["nlw"] = rep(g("norm_final_w"))
    shared["snwc"] = np.ascontiguousarray(g("ssd_norm_w")[0].reshape(16, 128).T)
    shared["sinks"] = rep(g("attn_sinks")[0])
    shared["convw"] = np.ascontiguousarray(g("conv_w")[0].reshape(4, 24, 128).transpose(2, 1, 0).reshape(128, 96))
    shared["convb"] = np.ascontiguousarray(g("conv_b")[0].reshape(24, 128).T)
    shared["dtb"] = rep(g("dt_bias")[0])
    shared["alog"] = rep(g("a_log")[0])
    shared["dskip"] = rep(g("d_skip")[0])
    maps = []
    xs = g("x")
    ps = g("positions")
    for c in range(ncores):
        m = dict(shared)
        m["x"] = np.ascontiguousarray(xs[c])
        m["pos"] = np.ascontiguousarray(ps[c].reshape(32, 128).T.astype(np.int32))
        maps.append(m)
    return maps


def kernel(**inputs):
    nc = build_program(32, debug=False)
    maps = make_in_maps(inputs, 8)
    res = run_bass_kernel_spmd(nc, maps, core_ids=list(range(8)))
    return np.stack([np.asarray(r["out"]) for r in res.results], axis=0).astype(np.float32)
```

```python
import numpy as np
import ml_dtypes
from contextlib import ExitStack
import concourse.bass as bass
import concourse.mybir as mybir
from concourse.bass_utils import run_bass_kernel_spmd

F32 = mybir.dt.float32
BF16 = mybir.dt.bfloat16
I32 = mybir.dt.int32
U32 = mybir.dt.uint32
AF = mybir.ActivationFunctionType
ALU = mybir.AluOpType
AX = mybir.AxisListType

D = 1024
SEQ = 4096
NEG = -30000.0
EPS = 1e-6
IN_DIM = 8736
TMW = 5664
ENGS = ["pe", "act", "dve", "pool", "sp"]
ARENA_WORDS = 45040


class Prog:
    def __init__(self, nc):
        self.nc = nc
        self.ops = {e: [] for e in ENGS}
        self.lastw = {}
        self.readers = {}
        self.dma_cnt = {}
        self.fence = {e: None for e in ENGS}

    def op(self, eng, fn, reads=(), writes=(), dma=False, semkey=None):
        idx = len(self.ops[eng])
        raw, other = set(), set()
        for k in reads:
            raw.update(self.lastw.get(k, {}).values())
        for k in writes:
            raw.update(self.lastw.get(k, {}).values())
            other.update(self.readers.get(k, {}).values())
        deps = set()
        for d in raw | other:
            de, di = d
            drec = self.ops[de][di]
            if de == eng and not drec["dma"] and not dma:
                if eng == "pe" or d not in raw:
                    continue
            deps.add(d)
        rec = dict(fn=fn, deps=deps, dma=dma, needed=False, inc=None, fence=self.fence[eng])
        self.fence[eng] = None
        if dma:
            c = self.dma_cnt.get(semkey, 0) + 1
            self.dma_cnt[semkey] = c
            rec["semkey"] = semkey
            rec["dma_val"] = 16 * c
            tk = ("d", eng, semkey)
        else:
            tk = ("c", eng)
        self.ops[eng].append(rec)
        me = (eng, idx)
        for k in reads:
            self.readers.setdefault(k, {})[tk] = me
        for k in writes:
            self.lastw.setdefault(k, {})[tk] = me
        return me

    def barrier(self):
        snap_c = {}
        for e in ENGS:
            for i in range(len(self.ops[e]) - 1, -1, -1):
                if not self.ops[e][i]["dma"]:
                    snap_c[e] = i
                    self.ops[e][i]["needed"] = True
                    break
        snap = (snap_c, dict(self.dma_cnt))
        for e in ENGS:
            self.fence[e] = snap

    def emit(self, out_semkeys=()):
        nc = self.nc
        for e in ENGS:
            for rec in self.ops[e]:
                for (de, di) in rec["deps"]:
                    d = self.ops[de][di]
                    if not d["dma"]:
                        d["needed"] = True
        for e in ENGS:
            c = 0
            for rec in self.ops[e]:
                if rec["needed"] and not rec["dma"]:
                    c += 1
                    rec["inc"] = c
        with ExitStack() as st:
            esem = {e: st.enter_context(nc.semaphore("sem_" + e)) for e in ENGS}
            dsem = {}
            for i, k in enumerate(self.dma_cnt):
                dsem[k] = st.enter_context(nc.semaphore("dsem_%d" % i))
            block = st.enter_context(nc.Block())
            prog = self

            def body(ename):
                def f(eng):
                    waited = {}

                    def w(key, s, v):
                        if waited.get(key, 0) >= v:
                            return
                        waited[key] = v
                        eng.wait_ge(s, v)

                    for rec in prog.ops[ename]:
                        if rec["fence"] is not None:
                            sc, sd = rec["fence"]
                            for fe, fi in sc.items():
                                w(("e", fe), esem[fe], prog.ops[fe][fi]["inc"])
                            for k, cnt in sd.items():
                                w(("d", k), dsem[k], 16 * cnt)
                        for (de, di) in sorted(rec["deps"]):
                            d = prog.ops[de][di]
                            if d["dma"]:
                                w(("d", d["semkey"]), dsem[d["semkey"]], d["dma_val"])
                            else:
                                w(("e", de), esem[de], d["inc"])
                        ins = rec["fn"](eng)
                        if rec["dma"]:
                            ins.then_inc(dsem[rec["semkey"]], 16)
                        elif rec["needed"]:
                            ins.then_inc(esem[ename], 1)
                    if ename == "sp":
                        for k in out_semkeys:
                            eng.wait_ge(dsem[k], 16 * prog.dma_cnt[k])
                return f

            block.tensor(body("pe"))
            block.scalar(body("act"))
            block.vector(body("dve"))
            block.gpsimd(body("pool"))
            block.sync(body("sp"))


class Arena:
    def __init__(self, base_ap, words):
        self.base = base_ap
        self.words = words
        self.off = 0

    def reset(self):
        self.off = 0

    def alloc(self, shape, dtype=F32):
        n = int(np.prod(shape))
        if dtype == BF16:
            w = (n + 1) // 2
        else:
            w = n
        w = (w + 7) // 8 * 8
        a = self.off
        self.off += w
        assert self.off <= self.words, "arena overflow %d > %d" % (self.off, self.words)
        v = self.base[:, a:a + w]
        if dtype != F32:
            v = v.bitcast(dtype)
        v = v[:, 0:n]
        if len(shape) == 2:
            v = v.rearrange("p (a b) -> p a b", b=shape[1])
        elif len(shape) == 3:
            v = v.rearrange("p (a b c) -> p a b c", b=shape[1], c=shape[2])
        return v


def build_program(NT=32, debug=False, phases=("A", "A2", "B1a", "B1b", "B2")):
    T = NT * 128
    nc = bass.Bass("TRN2", target_bir_lowering=False)

    def din(name, shape, dt=F32):
        return nc.dram_tensor(name, list(shape), dt, kind="ExternalInput").ap()

    skind = "ExternalOutput" if debug else "Internal"

    def dscr(name, shape, dt=F32):
        return nc.dram_tensor(name, list(shape), dt, kind=skind).ap()

    x = din("x", [SEQ, D])
    pos = din("pos", [128, 32], I32)
    w_in = din("w_in", [D, IN_DIM])
    w_attn_o = din("w_attn_o", [1024, D])
    w_ssd_o = din("w_ssd_o", [2048, D])
    w_out = din("w_out", [D, D])
    peer_wq = din("peer_wq", [D, 2048])
    keysT = din("keysT", [128, 16 * 128])
    peer_uv = din("peer_uv", [16384, 2 * D])
    nmw = din("nmw", [128, D])
    nfw = din("nfw", [128, D])
    nlw = din("nlw", [128, D])
    snwc = din("snwc", [128, 16])
    sinks = din("sinks", [128, 16])
    convw = din("convw", [128, 24 * 4])
    convb = din("convb", [128, 24])
    dtb = din("dtb", [128, 32])
    alog = din("alog", [128, 32])
    dskip = din("dskip", [128, 32])
    c_ident = din("c_ident", [128, 128], BF16)
    c_identf = din("c_identf", [128, 128])
    c_maskc = din("c_maskc", [128, 512], BF16)
    c_maskp = din("c_maskp", [128, 512], BF16)
    c_U = din("c_U", [128, 128])
    c_L = din("c_L", [128, 128])
    c_ones = din("c_ones", [128, 128])
    c_iota = din("c_iota", [128, 2048], BF16)
    c_invf = din("c_invf", [128, 8])
    out = nc.dram_tensor("out", [SEQ, D], F32, kind="ExternalOutput").ap()
    pt = dscr("pt", [SEQ, TMW])
    xbcT = dscr("xbcT", [3072, SEQ])
    xc = dscr("xc", [3072, SEQ])
    ma = dscr("ma", [SEQ, D])
    x1s = dscr("x1s", [SEQ, D])
    uvb = nc.dram_tensor("uvb", [16384, 2 * D], BF16, kind="Internal").ap()

    st = ExitStack()
    arena_t = st.enter_context(nc.sbuf_tensor("arena", [128, ARENA_WORDS], F32))
    AR = Arena(arena_t[:], ARENA_WORDS)
    PS = [st.enter_context(nc.psum_tensor("ps%d" % i, [128, 512], F32)) for i in range(8)]
    P = Prog(nc)

    def dma(q, out_ap, in_ap, reads, writes, semkey):
        P.op(q, lambda e: e.dma_start(out=out_ap, in_=in_ap), reads=reads, writes=writes,
             dma=True, semkey=semkey)

    def mm(out_ap, lhsT, rhs, start, stop, reads, writes, skip=False):
        P.op("pe", lambda e: e.matmul(out_ap, lhsT, rhs, start=start, stop=stop,
                                      skip_group_check=skip), reads=reads, writes=writes)

    def tr(out_ap, in_ap, ident, reads, writes):
        P.op("pe", lambda e: e.transpose(out_ap, in_ap, ident), reads=reads, writes=writes)

    def act(out_ap, in_ap, func, reads, writes, bias=None, scale=None, accum_out=None):
        kw = {}
        if bias is not None:
            kw["bias"] = bias
        if scale is not None:
            kw["scale"] = scale
        if accum_out is not None:
            kw["accum_out"] = accum_out
        P.op("act", lambda e: e.activation(out_ap, in_ap, func, **kw), reads=reads, writes=writes)

    def tt(eng, out_ap, in0, in1, op, reads, writes):
        P.op(eng, lambda e: e.tensor_tensor(out_ap, in0, in1, op), reads=reads, writes=writes)

    def ts(eng, out_ap, in0, s1, s2, op0, op1, reads, writes, accum_out=None):
        if op1 is None:
            P.op(eng, lambda e: e.tensor_scalar(out_ap, in0, s1, None, op0), reads=reads, writes=writes)
        else:
            P.op(eng, lambda e: e.tensor_scalar(out_ap, in0, s1, s2, op0, op1, accum_out=accum_out),
                 reads=reads, writes=writes)

    def stt(out_ap, in0, scalar, in1, op0, op1, reads, writes, accum_out=None):
        P.op("dve", lambda e: e.scalar_tensor_tensor(out_ap, in0, scalar, in1, op0, op1,
                                                     accum_out=accum_out), reads=reads, writes=writes)

    def cp(eng, out_ap, in_ap, reads, writes):
        if eng == "act":
            P.op("act", lambda e: e.copy(out_ap, in_ap), reads=reads, writes=writes)
        else:
            P.op(eng, lambda e: e.tensor_copy(out_ap, in_ap), reads=reads, writes=writes)

    def rstd_from_ss(rs, n, key):
        ts("dve", rs, rs, 1.0 / n, EPS, ALU.mult, ALU.add, [key], [key])
        act(rs, rs, AF.Sqrt, [key], [key])
        P.op("dve", lambda e: e.reciprocal(rs, rs), reads=[key], writes=[key])

    def psb(i):
        return PS[i][:]

    def psb16(i):
        return PS[i][:].bitcast(BF16)


    CVW = 4096
    cvreg = arena_t[:, ARENA_WORDS - CVW:ARENA_WORDS].bitcast(BF16)
    cvb = [cvreg[:, s_ * 2048:(s_ + 1) * 2048] for s_ in range(4)]
    cv_state = [0, 0]
    NCH = 128

    def conv_steps(k):
        for _ in range(k):
            c = cv_state[0]
            if c < NCH:
                sidx = c % 4
                dma("pool", cvb[sidx], peer_uv[c * 128:(c + 1) * 128, :], [], ["cv%d" % sidx], "cvl%d" % sidx)
                cv_state[0] += 1
            if cv_state[0] - cv_state[1] > 2 or (cv_state[0] == NCH and cv_state[1] < NCH):
                c2 = cv_state[1]
                sidx = c2 % 4
                dma("pool", uvb[c2 * 128:(c2 + 1) * 128, :], cvb[sidx], ["cv%d" % sidx], [], "cvs%d" % sidx)
                cv_state[1] += 1

    def conv_flush():
        while cv_state[1] < NCH:
            conv_steps(1)

    if "A" in phases:
        AR.reset()
        wtm = AR.alloc([8, TMW], BF16)
        wfm = AR.alloc([8, 3072], BF16)
        nw = AR.alloc([D])
        ident = AR.alloc([128], BF16)
        xt = [AR.alloc([D]) for _ in range(2)]
        hb = [AR.alloc([D], BF16) for _ in range(2)]
        hT = [AR.alloc([8, 128], BF16) for _ in range(2)]
        stg = [AR.alloc([512]) for _ in range(4)]
        ssq = [AR.alloc([8]) for _ in range(2)]
        dma("sp", nw, nmw, [], ["nw"], "nw")
        dma("sp", ident, c_ident, [], ["ident"], "ident")
        pieces_tm = [(0, 1792, 0), (1792, 3584, 1792), (6656, 7696, 3584), (7696, 8736, 4624)]
        pieces_fm = [(3584, 5120, 0), (5120, 6656, 1536)]
        for kc in range(8):
            for (c0, c1, d0) in pieces_tm:
                dma("pool", wtm[:, kc, d0:d0 + (c1 - c0)], w_in[kc * 128:(kc + 1) * 128, c0:c1],
                    [], ["wtm"], "wtm")
            for (c0, c1, d0) in pieces_fm:
                dma("pool", wfm[:, kc, d0:d0 + (c1 - c0)], w_in[kc * 128:(kc + 1) * 128, c0:c1],
                    [], ["wfm"], "wfm")
        bank_rr = [0]
        stg_rr = [0]
        for i in range(NT):
            b = i % 2
            kx, kh, khT, ks = "xt%d" % b, "hb%d" % b, "hT%d" % b, "ssq%d" % b
            dma("sp", xt[b], x[i * 128:(i + 1) * 128, :], [], [kx], kx)
            rs = ssq[b][:, 0:1]
            stt(hb[b], xt[b], 1.0, xt[b], ALU.mult, ALU.mult, [kx], [kh, ks], accum_out=rs)
            rstd_from_ss(rs, D, ks)
            stt(hb[b], xt[b], rs, nw, ALU.mult, ALU.mult, [kx, ks, "nw"], [kh])
            pst = "psT%d" % b
            for kc in range(8):
                tr(psb16(b)[:, kc * 128:(kc + 1) * 128], hb[b][:, kc * 128:(kc + 1) * 128], ident,
                   [kh, "ident"], [pst])
            cp("act", hT[b], psb16(b).rearrange("p (a b) -> p a b", b=128), [pst], [khT])
            ngrp = (TMW + 511) // 512
            for cg in range(ngrp):
                c0 = cg * 512
                cw = min(512, TMW - c0)
                bk = 2 + bank_rr[0] % 6
                bank_rr[0] += 1
                kb = "psb%d" % bk
                for kc in range(8):
                    mm(psb(bk)[:, 0:cw], hT[b][:, kc, :], wtm[:, kc, c0:c0 + cw], kc == 0, kc == 7,
                       [khT, "wtm"], [kb])
                s = stg_rr[0] % 4
                stg_rr[0] += 1
                ksg = "stg%d" % s
                cp("act" if cg % 2 == 0 else "dve", stg[s][:, 0:cw], psb(bk)[:, 0:cw], [kb], [ksg])
                dma("sp", pt[i * 128:(i + 1) * 128, c0:c0 + cw], stg[s][:, 0:cw], [ksg], [], ksg)
            for grp in range(6):
                bk = 2 + bank_rr[0] % 6
                bank_rr[0] += 1
                kb = "psb%d" % bk
                for j in range(4):
                    ch = grp * 4 + j
                    for kc in range(8):
                        mm(psb(bk)[:, j * 128:(j + 1) * 128], wfm[:, kc, ch * 128:(ch + 1) * 128],
                           hT[b][:, kc, :], kc == 0, kc == 7, [khT, "wfm"], [kb])
                s = stg_rr[0] % 4
                stg_rr[0] += 1
                ksg = "stg%d" % s
                cp("act" if grp % 2 == 0 else "dve", stg[s], psb(bk), [kb], [ksg])
                dst = xbcT[grp * 512:(grp + 1) * 512, i * 128:(i + 1) * 128].rearrange(
                    "(j p) t -> p j t", p=128)
                dma("sp", dst, stg[s].rearrange("p (j t) -> p j t", t=128), [ksg], [], ksg)
        P.barrier()

    if "A2" in phases:
        AR.reset()
        cw_sb = AR.alloc([24, 4])
        cb_sb = AR.alloc([24])
        u = [AR.alloc([T + 8]) for _ in range(2)]
        acc = [AR.alloc([T]) for _ in range(2)]
        dma("sp", cw_sb, convw.rearrange("p (c j) -> p c j", j=4), [], ["cw"], "cw")
        dma("sp", cb_sb, convb, [], ["cb"], "cb")
        for b in range(2):
            P.op("dve", lambda e, b=b: e.memset(u[b][:, 0:3], 0.0), writes=["u%d" % b])
        for ch in range(24):
            b = ch % 2
            ku, ka = "u%d" % b, "acc%d" % b
            dma("sp", u[b][:, 3:3 + T], xbcT[ch * 128:(ch + 1) * 128, 0:T], [], [ku], ku)
            ts("dve", acc[b], u[b][:, 0:T], cw_sb[:, ch, 0:1], None, ALU.mult, None, [ku, "cw"], [ka])
            for j in range(1, 4):
                stt(acc[b], u[b][:, j:j + T], cw_sb[:, ch, j:j + 1], acc[b], ALU.mult, ALU.add,
                    [ku, "cw", ka], [ka])
            act(acc[b], acc[b], AF.Silu, [ka, "cb"], [ka], bias=cb_sb[:, ch:ch + 1])
            dma("sp", xc[ch * 128:(ch + 1) * 128, 0:T], acc[b], [ka], [], ka)
            if "B2" in phases:
                conv_steps(2)
        assert AR.off <= ARENA_WORDS - CVW
        P.barrier()


    def run_pipelined(make_gen, n_tiles, start_every):
        active = []
        nxt = 0
        rnd = 0
        while nxt < n_tiles or active:
            if nxt < n_tiles and rnd % start_every == 0:
                active.append(make_gen(nxt))
                nxt += 1
            still = []
            for g_ in active:
                try:
                    next(g_)
                    still.append(g_)
                except StopIteration:
                    pass
            active = still
            rnd += 1


    TWO_PI = 2.0 * np.pi

    def bc(ap, shape):
        return ap.to_broadcast(list(shape))

    if "B1a" in phases:
        AR.reset()
        wao = AR.alloc([8, 1024], BF16)
        ident = AR.alloc([128], BF16)
        maskc = AR.alloc([512], BF16)
        maskp = AR.alloc([512], BF16)
        posi = AR.alloc([32], I32)
        posf = AR.alloc([32])
        invf = AR.alloc([8])
        ang = AR.alloc([32, 8])
        cs = AR.alloc([32, 8])
        sn = AR.alloc([32, 8])
        ra = AR.alloc([32, 8])
        rb_ = AR.alloc([32, 8])
        rc = AR.alloc([32, 8])
        rki = AR.alloc([32, 8], I32)
        esink = AR.alloc([16])
        qkv = [AR.alloc([1536]) for _ in range(2)]
        gt = [AR.alloc([1024]) for _ in range(2)]
        qk_bf = [AR.alloc([20, 64], BF16) for _ in range(2)]
        kd = [AR.alloc([4, 2, 128], BF16) for _ in range(2)]
        rt = [[AR.alloc([20, 8]) for _ in range(4)] for _ in range(2)]
        qT = [AR.alloc([8, 128], BF16) for _ in range(2)]
        kT = [AR.alloc([8, 128], BF16) for _ in range(3)]
        v1 = [AR.alloc([4, 66], BF16) for _ in range(3)]
        pTc = [[AR.alloc([512], BF16) for _ in range(2)] for _ in range(2)]
        pTp = [[AR.alloc([512], BF16) for _ in range(2)] for _ in range(2)]
        den = [[AR.alloc([4]) for _ in range(2)] for _ in range(2)]
        attn = [AR.alloc([16, 64], BF16) for _ in range(2)]
        attnT = [AR.alloc([8, 128], BF16) for _ in range(2)]
        gsig = [AR.alloc([1024]) for _ in range(2)]
        mres = [AR.alloc([1024]) for _ in range(2)]
        for kc in range(8):
            dma("pool", wao[:, kc, :], w_attn_o[kc * 128:(kc + 1) * 128, :], [], ["wao"], "wao")
        dma("sp", ident, c_ident, [], ["ident"], "identB")
        dma("sp", maskc, c_maskc, [], ["maskc"], "maskc")
        dma("sp", maskp, c_maskp, [], ["maskp"], "maskp")
        dma("sp", posi, pos, [], ["posi"], "posi")
        dma("sp", invf, c_invf, [], ["invf"], "invf")
        dma("sp", esink, sinks, [], ["esink"], "esink")
        act(esink, esink, AF.Exp, ["esink"], ["esink"])
        cp("dve", posf, posi, ["posi"], ["posf"])
        tt("dve", ang, bc(posf.unsqueeze(2), [128, 32, 8]), bc(invf.unsqueeze(1), [128, 32, 8]), ALU.mult,
           ["posf", "invf"], ["ang"])

        def sin_of(dst, dkey, shift):
            ts("dve", ra, ang, shift, 1.0 / TWO_PI, ALU.add, ALU.mult, ["ang"], ["ra"])
            cp("dve", rki, ra, ["ra"], ["rki"])
            cp("dve", rb_, rki, ["rki"], ["rb"])
            ts("dve", ra, ang, shift, None, ALU.add, None, ["ang"], ["ra"])
            stt(ra, rb_, -TWO_PI, ra, ALU.mult, ALU.add, ["rb", "ra"], ["ra"])
            ts("dve", rb_, ra, float(np.pi), None, ALU.is_gt, None, ["ra"], ["rb"])
            ts("dve", rc, ra, float(-np.pi), None, ALU.is_lt, None, ["ra"], ["rc"])
            tt("dve", rc, rc, rb_, ALU.subtract, ["rb", "rc"], ["rc"])
            stt(ra, rc, TWO_PI, ra, ALU.mult, ALU.add, ["rc", "ra"], ["ra"])
            ts("dve", ra, ra, float(np.pi), float(-np.pi), ALU.min, ALU.max, ["ra"], ["ra"])
            act(dst, ra, AF.Sin, ["ra"], [dkey])

        sin_of(sn, "sn", 0.0)
        sin_of(cs, "cs", float(np.pi / 2))
        for b in range(3):
            P.op("dve", lambda e, b=b: e.memset(v1[b][:, :, 64:66], 1.0), writes=["v1_%d" % b])
        for b in range(2):
            P.op("pool", lambda e, b=b: e.memset(kd[b], 0.0), writes=["kd%d" % b])

        def b1a_tile(i):
            b = i % 2
            b3 = i % 3
            pb3 = (i - 1) % 3
            S = lambda n: "%s_%d" % (n, b)
            kq, kg = "qkv%d" % b, "gt%d" % b
            rows = slice(i * 128, (i + 1) * 128)
            dma("sp", qkv[b], pt[rows, 0:1536], [], [kq], kq)
            dma("sp", gt[b], pt[rows, 3616:4640], [], [kg], kg)
            qk = qkv[b][:, 0:1280].rearrange("p (h d) -> p h d", d=64)
            x1 = qk[:, :, 0:8]
            x2 = qk[:, :, 8:16]
            cb_ = bc(cs[:, i:i + 1, :], [128, 20, 8])
            sb_ = bc(sn[:, i:i + 1, :], [128, 20, 8])
            r_ = rt[b]
            qb = qk_bf[b]
            tt("dve", r_[0], x1, cb_, ALU.mult, [kq, "cs"], [S("rt0")])
            tt("dve", r_[1], x2, sb_, ALU.mult, [kq, "sn"], [S("rt1")])
            tt("dve", qb[:, :, 0:8], r_[0], r_[1], ALU.subtract, [S("rt0"), S("rt1")], [S("qkbf")])
            tt("dve", r_[2], x2, cb_, ALU.mult, [kq, "cs"], [S("rt2")])
            tt("dve", r_[3], x1, sb_, ALU.mult, [kq, "sn"], [S("rt3")])
            tt("dve", qb[:, :, 8:16], r_[2], r_[3], ALU.add, [S("rt2"), S("rt3")], [S("qkbf")])
            cp("pool", qb[:, :, 16:64], qk[:, :, 16:64], [kq], [S("qkbf2")])
            cp("pool", kd[b][:, :, 0, 0:64], qb[:, 16:20, :], [S("qkbf"), S("qkbf2")], ["kd%d" % b])
            cp("pool", kd[b][:, :, 1, 64:128], qb[:, 16:20, :], [S("qkbf"), S("qkbf2")], ["kd%d" % b])
            cp("act", v1[b3][:, :, 0:64], qkv[b][:, 1280:1536].rearrange("p (h d) -> p h d", d=64),
               [kq], ["v1_%d" % b3])
            kpsT = "ps%d" % b
            for j in range(8):
                tr(psb16(b)[:, j * 128:(j + 1) * 128],
                   qb[:, 2 * j:2 * j + 2, :].rearrange("p a b -> p (a b)"), ident,
                   [S("qkbf"), S("qkbf2"), "ident"], [kpsT])
            cp("act", qT[b], psb16(b).rearrange("p (a b) -> p a b", b=128), [kpsT], [S("qT")])
            for h2 in range(8):
                tr(psb16(b)[:, h2 * 128:(h2 + 1) * 128], kd[b][:, h2 // 2, h2 % 2, :], ident,
                   ["kd%d" % b, "ident"], [kpsT])
            cp("dve", kT[b3], psb16(b).rearrange("p (a b) -> p a b", b=128), [kpsT], ["kT%d" % b3])
            yield
            for h in range(4):
                hp = h % 2
                bc_, bp_, bo_ = 2 + hp * 2, 3 + hp * 2, 6 + hp
                kbc, kbp, kbo = "ps%d" % bc_, "ps%d" % bp_, "ps%d" % bo_
                pc, pp = pTc[b][hp], pTp[b][hp]
                kpc, kpp = "pTc%d_%d" % (b, hp), "pTp%d_%d" % (b, hp)
                blocks = [(bc_, kbc, maskc, "maskc", kT[b3], "kT%d" % b3, pc, kpc)]
                if i > 0:
                    blocks.append((bp_, kbp, maskp, "maskp", kT[pb3], "kT%d" % pb3, pp, kpp))
                for (bk, kb, msk, kmsk, kTt, kkT, pT_, kpT) in blocks:
                    for g in range(4):
                        half = g % 2
                        mm(psb(bk)[:, g * 128:(g + 1) * 128], ident, msk[:, 0:128], True, False,
                           ["ident", kmsk], [kb])
                        mm(psb(bk)[:, g * 128:(g + 1) * 128], kTt[:, 2 * h + half, :],
                           qT[b][:, 2 * h + g // 2, :], False, True,
                           [kkT, S("qT")], [kb])
                    act(pT_, psb(bk), AF.Exp, [kb], [kpT], scale=0.125)
                for g in range(4):
                    o_ap = psb(bo_)[:, g * 65:(g + 1) * 65]
                    if i > 0:
                        mm(o_ap, pp[:, g * 128:(g + 1) * 128], v1[pb3][:, h, 0:65], True, False,
                           [kpp, "v1_%d" % pb3], [kbo])
                        mm(o_ap, pc[:, g * 128:(g + 1) * 128], v1[b3][:, h, 0:65], False, True,
                           [kpc, "v1_%d" % b3], [kbo])
                    else:
                        mm(o_ap, pc[:, g * 128:(g + 1) * 128], v1[b3][:, h, 0:65], True, True,
                           [kpc, "v1_%d" % b3], [kbo])
                ov = psb(bo_)[:, 0:260].rearrange("p (g d) -> p g d", d=65)
                dn = den[b][hp]
                kden = "den%d_%d" % (b, hp)
                tt("dve", dn, ov[:, :, 64], esink[:, 4 * h:4 * h + 4], ALU.add, [kbo, "esink"], [kden])
                P.op("dve", lambda e, dn=dn: e.reciprocal(dn, dn), reads=[kden], writes=[kden])
                tt("dve", attn[b][:, 4 * h:4 * h + 4, :], ov[:, :, 0:64], bc(dn.unsqueeze(2), [128, 4, 64]),
                   ALU.mult, [kbo, kden], [S("attn")])
                yield
            for j in range(8):
                tr(psb16(b)[:, j * 128:(j + 1) * 128],
                   attn[b][:, 2 * j:2 * j + 2, :].rearrange("p a b -> p (a b)"), ident, [S("attn"), "ident"], [kpsT])
            cp("act", attnT[b], psb16(b).rearrange("p (a b) -> p a b", b=128), [kpsT], [S("attnT")])
            act(gsig[b], gt[b], AF.Sigmoid, [kg], [S("gsig")])
            for cg in range(2):
                bk = 2 + cg + 2 * b
                for kc in range(8):
                    mm(psb(bk), attnT[b][:, kc, :], wao[:, kc, cg * 512:(cg + 1) * 512], kc == 0, kc == 7,
                       [S("attnT"), "wao"], ["ps%d" % bk])
                tt("dve", mres[b][:, cg * 512:(cg + 1) * 512], psb(bk), gsig[b][:, cg * 512:(cg + 1) * 512],
                   ALU.mult, ["ps%d" % bk, S("gsig")], ["mres%d" % b])
            dma("sp", ma[rows, :], mres[b], ["mres%d" % b], [], "mres%d" % b)
            if "B2" in phases:
                conv_steps(3)
            yield

        run_pipelined(b1a_tile, NT, 3)
        if "B2" in phases:
            conv_flush()
        assert AR.off <= ARENA_WORDS - CVW
        P.barrier()

    if "B1b" in phases:
        AR.reset()
        wso = AR.alloc([16, 1024], BF16)
        wo = AR.alloc([8, 1024], BF16)
        ident = AR.alloc([128], BF16)
        identf = AR.alloc([128])
        maskc = AR.alloc([512], BF16)
        Um = AR.alloc([128])
        Lm = AR.alloc([128])
        onesm = AR.alloc([128])
        snw_sb = AR.alloc([16])
        dtb_sb = AR.alloc([32])
        A_sb = AR.alloc([32])
        dsk_sb = AR.alloc([32])
        one_c = AR.alloc([8])
        stateT = AR.alloc([4, 512])
        stateT_bf = AR.alloc([4, 512], BF16)
        zt = [AR.alloc([2048]) for _ in range(2)]
        dtr = [AR.alloc([32]) for _ in range(2)]
        g2 = [AR.alloc([1024]) for _ in range(2)]
        xct = [AR.alloc([24, 128]) for _ in range(2)]
        mat = [AR.alloc([1024]) for _ in range(2)]
        xt = [AR.alloc([1024]) for _ in range(2)]
        dtt = [AR.alloc([32]) for _ in range(2)]
        dA = [AR.alloc([32]) for _ in range(2)]
        acum_sb = [AR.alloc([32]) for _ in range(2)]
        ea = [AR.alloc([32]) for _ in range(2)]
        wd = [AR.alloc([32]) for _ in range(2)]
        etot = [AR.alloc([32]) for _ in range(2)]
        wdt = [AR.alloc([32]) for _ in range(2)]
        BCbf = [AR.alloc([8, 128], BF16) for _ in range(2)]
        B_tm = [AR.alloc([4, 128], BF16) for _ in range(2)]
        ynT = [AR.alloc([16, 128], BF16) for _ in range(2)]
        xdt = AR.alloc([8, 64], BF16)
        xdtw = AR.alloc([8, 64], BF16)
        xsD = AR.alloc([8, 64], BF16)
        cbT = AR.alloc([128])
        Rm = [AR.alloc([4, 128]) for _ in range(2)]
        decT = [AR.alloc([4, 128]) for _ in range(2)]
        MT = [AR.alloc([4, 128], BF16) for _ in range(2)]
        ytmp = AR.alloc([8, 64])
        yg = AR.alloc([8, 64])
        ssg = AR.alloc([8])
        yn_g = AR.alloc([512], BF16)
        mtmp = AR.alloc([1024])
        mbf = AR.alloc([1024], BF16)
        mT = AR.alloc([8, 128], BF16)
        if debug:
            print("B1b arena words", AR.off)
        dma("sp", snw_sb, snwc, [], ["snw"], "snw")
        for kc in range(16):
            sb = kc % 2
            stg_ = zt[sb][:, 0:1024]
            dma("sp", stg_, w_ssd_o[kc * 128:(kc + 1) * 128, :], [], ["zt%d" % sb], "zt%d" % sb)
            ts("dve", wso[:, kc, :], stg_, snw_sb[:, kc:kc + 1], None, ALU.mult, None,
               ["zt%d" % sb, "snw"], ["wso"])
        for kc in range(8):
            dma("pool", wo[:, kc, :], w_out[kc * 128:(kc + 1) * 128, :], [], ["wo"], "wo")
        dma("sp", ident, c_ident, [], ["ident"], "identC")
        dma("sp", identf, c_identf, [], ["identf"], "identf")
        dma("sp", maskc, c_maskc, [], ["maskc"], "maskcC")
        dma("sp", Um, c_U, [], ["Um"], "Um")
        dma("sp", Lm, c_L, [], ["Lm"], "Lm")
        dma("sp", onesm, c_ones, [], ["onesm"], "onesm")
        dma("sp", dtb_sb, dtb, [], ["dtb"], "dtb")
        dma("sp", A_sb, alog, [], ["A"], "A")
        dma("sp", dsk_sb, dskip, [], ["dsk"], "dsk")
        act(A_sb, A_sb, AF.Exp, ["A"], ["A"])
        ts("dve", A_sb, A_sb, -1.0, None, ALU.mult, None, ["A"], ["A"])
        P.op("dve", lambda e: e.memset(one_c, 1.0), writes=["one_c"])
        P.op("dve", lambda e: e.memset(stateT, 0.0), writes=["stateT"])
        P.op("dve", lambda e: e.memset(stateT_bf, 0.0), writes=["stateTbf"])
        rrb = [0]

        def nbank():
            bk = rrb[0] % 8
            rrb[0] += 1
            return bk, "ps%d" % bk

        def b1b_tile(i):
            b = i % 2
            S = lambda n: "%s_%d" % (n, b)
            rows = slice(i * 128, (i + 1) * 128)
            kzt, kdtr, kg2, kxct, kmat, kxt = ("zt%d" % b, "dtr%d" % b, "g2_%d" % b, "xct%d" % b,
                                               "mat%d" % b, "xtB%d" % b)
            dma("sp", dtr[b], pt[rows, 3584:3616], [], [kdtr], kdtr)
            dma("sp", xct[b], xc[:, i * 128:(i + 1) * 128].rearrange("(c p) t -> p c t", p=128), [], [kxct], kxct)
            dma("sp", zt[b], pt[rows, 1536:3584], [], [kzt], kzt)
            dma("sp", g2[b], pt[rows, 4640:5664], [], [kg2], kg2)
            dma("sp", mat[b], ma[rows, :], [], [kmat], kmat)
            dma("sp", xt[b], x[rows, :], [], [kxt], kxt)
            tt("dve", dtt[b], dtr[b], dtb_sb, ALU.add, [kdtr, "dtb"], [S("dtt")])
            act(dtt[b], dtt[b], AF.Exp, [S("dtt")], [S("dtt")])
            act(dtt[b], dtt[b], AF.Ln, [S("dtt"), "one_c"], [S("dtt")], bias=one_c[:, 0:1])
            tt("dve", dA[b], dtt[b], A_sb, ALU.mult, [S("dtt"), "A"], [S("dA")])
            bk, kb = nbank()
            mm(psb(bk)[:, 0:32], Um, dA[b], True, True, ["Um", S("dA")], [kb])
            mm(psb(bk)[:, 32:64], onesm, dA[b], True, True, ["onesm", S("dA")], [kb])
            cp("dve", acum_sb[b], psb(bk)[:, 0:32], [kb], [S("acum")])
            act(ea[b], psb(bk)[:, 0:32], AF.Exp, [kb], [S("ea")])
            act(etot[b], psb(bk)[:, 32:64], AF.Exp, [kb], [S("etot")])
            tt("dve", wd[b], psb(bk)[:, 32:64], acum_sb[b], ALU.subtract, [kb, S("acum")], [S("wd")])
            act(wd[b], wd[b], AF.Exp, [S("wd")], [S("wd")])
            tt("dve", wdt[b], wd[b], dtt[b], ALU.mult, [S("wd"), S("dtt")], [S("wdt")])
            cp("act", BCbf[b], xct[b][:, 16:24, :], [kxct], [S("BCbf")])
            bk, kb = nbank()
            for g in range(4):
                tr(psb16(bk)[:, g * 128:(g + 1) * 128], BCbf[b][:, g, :], ident, [S("BCbf"), "ident"], [kb])
            cp("act", B_tm[b], psb16(bk)[:, 0:512].rearrange("p (a b) -> p a b", b=128), [kb], [S("B_tm")])
            yield
            for g in range(4):
                hs = slice(8 * g, 8 * g + 8)
                bkx, kx = nbank()
                for j in range(4):
                    tr(psb(bkx)[:, j * 128:(j + 1) * 128], xct[b][:, 4 * g + j, :], identf, [kxct, "identf"], [kx])
                xv = psb(bkx).rearrange("p (h d) -> p h d", d=64)
                tt("dve", xdt, xv, bc(dtt[b][:, hs].unsqueeze(2), [128, 8, 64]), ALU.mult, [kx, S("dtt")], ["xdt"])
                tt("dve", xdtw, xv, bc(wdt[b][:, hs].unsqueeze(2), [128, 8, 64]), ALU.mult, [kx, S("wdt")], ["xdtw"])
                tt("dve", xsD, xv, bc(dsk_sb[:, hs].unsqueeze(2), [128, 8, 64]), ALU.mult, [kx, "dsk"], ["xsD"])
                bk, kb = nbank()
                mm(psb(bk)[:, 0:128], BCbf[b][:, g, :], BCbf[b][:, 4 + g, :], True, True, [S("BCbf")], [kb])
                cp("act", cbT, psb(bk)[:, 0:128], [kb], ["cbT"])
                bky, kby = nbank()
                for blk in range(2):
                    h0 = 8 * g + 4 * blk
                    rb2 = blk
                    tt("pool", Rm[rb2], bc(Um.unsqueeze(1), [128, 4, 128]),
                       bc(dA[b][:, h0:h0 + 4].unsqueeze(2), [128, 4, 128]), ALU.mult, ["Um", S("dA")], ["Rm%d" % rb2])
                    bkd, kbd = nbank()
                    mm(psb(bkd), ident, maskc, True, False, ["ident", "maskc"], [kbd])
                    mm(psb(bkd), Lm, Rm[rb2].rearrange("p a b -> p (a b)"), False, True, ["Lm", "Rm%d" % rb2], [kbd])
                    act(decT[rb2], psb(bkd).rearrange("p (a b) -> p a b", b=128), AF.Exp, [kbd], ["decT%d" % rb2])
                    tt("dve", MT[rb2], decT[rb2], bc(cbT.unsqueeze(1), [128, 4, 128]), ALU.mult,
                       ["decT%d" % rb2, "cbT"], ["MT%d" % rb2])
                    for j in range(4):
                        hh = 4 * blk + j
                        mm(psb(bky)[:, hh * 64:(hh + 1) * 64], ident, xsD[:, hh, :], True, False,
                           ["ident", "xsD"], [kby])
                        mm(psb(bky)[:, hh * 64:(hh + 1) * 64], MT[rb2][:, j, :], xdt[:, hh, :], False, True,
                           ["MT%d" % rb2, "xdt"], [kby])
                bki, kbi = nbank()
                mm(psb(bki), BCbf[b][:, 4 + g, :], stateT_bf[:, g, :], True, True, [S("BCbf"), "stateTbf"], [kbi])
                tt("dve", ytmp, psb(bki).rearrange("p (h d) -> p h d", d=64),
                   bc(ea[b][:, hs].unsqueeze(2), [128, 8, 64]), ALU.mult, [kbi, S("ea")], ["ytmp"])
                tt("dve", yg, psb(bky).rearrange("p (h d) -> p h d", d=64), ytmp, ALU.add, [kby, "ytmp"], ["yg"])
                bks, kbs = nbank()
                mm(psb(bks), B_tm[b][:, g, :], xdtw.rearrange("p a b -> p (a b)"), True, True,
                   [S("B_tm"), "xdtw"], [kbs])
                sv = stateT[:, g, :].rearrange("p (h d) -> p h d", d=64)
                tt("pool", sv, sv, bc(etot[b][:, hs].unsqueeze(2), [128, 8, 64]), ALU.mult, ["stateT", S("etot")], ["stateT"])
                tt("dve", stateT[:, g, :], stateT[:, g, :], psb(bks), ALU.add, ["stateT", kbs], ["stateT"])
                cp("act", stateT_bf[:, g, :], stateT[:, g, :], ["stateT"], ["stateTbf"])
                zg = zt[b][:, g * 512:(g + 1) * 512]
                act(zg, zg, AF.Silu, [kzt], [kzt])
                ygf = yg.rearrange("p a b -> p (a b)")
                tt("dve", ygf, ygf, zg, ALU.mult, ["yg", kzt], ["yg"])
                rs = ssg[:, g:g + 1]
                stt(ytmp.rearrange("p a b -> p (a b)"), ygf, 1.0, ygf, ALU.mult, ALU.mult, ["yg"], ["ytmp", "ssg"], accum_out=rs)
                rstd_from_ss(rs, 512, "ssg")
                ts("dve", yn_g, ygf, rs, None, ALU.mult, None, ["yg", "ssg"], ["yn_g"])
                bk, kb = nbank()
                for j in range(4):
                    tr(psb16(bk)[:, j * 128:(j + 1) * 128], yn_g[:, j * 128:(j + 1) * 128], ident, ["yn_g", "ident"], [kb])
                cp("act", ynT[b][:, 4 * g:4 * g + 4, :], psb16(bk)[:, 0:512].rearrange("p (a b) -> p a b", b=128), [kb], [S("ynT")])
                yield
            act(g2[b], g2[b], AF.Sigmoid, [kg2], [kg2])
            for cg in range(2):
                bk, kb = nbank()
                cs_ = slice(cg * 512, (cg + 1) * 512)
                for kc in range(16):
                    mm(psb(bk), ynT[b][:, kc, :], wso[:, kc, cs_], kc == 0, kc == 15, [S("ynT"), "wso"], [kb])
                tt("dve", mtmp[:, cs_], psb(bk), g2[b][:, cs_], ALU.mult, [kb, kg2], ["mtmp"])
                tt("dve", mbf[:, cs_], mtmp[:, cs_], mat[b][:, cs_], ALU.add, ["mtmp", kmat], ["mbf"])
            bk, kb = nbank()
            for j in range(8):
                tr(psb16(bk)[:, j * 128:(j + 1) * 128], mbf[:, j * 128:(j + 1) * 128], ident, ["mbf", "ident"], [kb])
            cp("act", mT, psb16(bk).rearrange("p (a b) -> p a b", b=128), [kb], ["mT"])
            for cg in range(2):
                bk, kb = nbank()
                cs_ = slice(cg * 512, (cg + 1) * 512)
                for kc in range(8):
                    mm(psb(bk), mT[:, kc, :], wo[:, kc, cs_], kc == 0, kc == 7, ["mT", "wo"], [kb])
                tt("dve", mtmp[:, cs_], psb(bk), xt[b][:, cs_], ALU.add, [kb, kxt], ["mtmp"])
            dma("sp", x1s[rows, :], mtmp, ["mtmp"], [], "mtmp")
            yield

        run_pipelined(b1b_tile, NT, 3)
        P.barrier()

    if "B2" in phases:
        if "A2" not in phases or "B1a" not in phases:
            conv_flush()
            P.barrier()
        AR.reset()
        NB = 14
        GS = 4
        wq = AR.alloc([8, 2048], BF16)
        kTs = AR.alloc([16, 128], BF16)
        ident = AR.alloc([128], BF16)
        identf = AR.alloc([128])
        nfw_sb = AR.alloc([1024])
        nlw_sb = AR.alloc([1024])
        iota_sb = AR.alloc([128, 16], BF16)
        x1t = [AR.alloc([1024]) for _ in range(2)]
        hx = AR.alloc([1024])
        hbf = [AR.alloc([1024], BF16) for _ in range(2)]
        prod = [AR.alloc([1024], BF16) for _ in range(4)]
        junkA = AR.alloc([1024], BF16)
        hnT = AR.alloc([8, 128], BF16)
        qpT = AR.alloc([16, 128], BF16)
        s_all = AR.alloc([16, 128])
        work = AR.alloc([2048])
        s_w = work.rearrange("p (a b) -> p a b", b=128)
        cand_w = work.rearrange("p (a b) -> p a b", b=256)
        oh = work.rearrange("p (a b) -> p a b", b=16)
        vals = AR.alloc([16, 16])
        idxu = AR.alloc([16, 16], U32)
        i12f = AR.alloc([16, 16])
        cand = s_all.rearrange("p a b -> p (a b)").rearrange("p (a b) -> p a b", b=256)
        sc = AR.alloc([8, 16])
        flat = AR.alloc([8, 16], U32)
        fab = AR.alloc([8, 16], U32)
        fabf = AR.alloc([8, 16])
        e1f = AR.alloc([128])
        e2f = AR.alloc([128])
        idxf = AR.alloc([128])
        idx_i = [AR.alloc([128], I32) for _ in range(2)]
        es = AR.alloc([8, 16])
        ssum = AR.alloc([8])
        gsm = [AR.alloc([128]) for _ in range(2)]
        a_all = [AR.alloc([128]) for _ in range(2)]
        w_all = AR.alloc([128])
        w2 = AR.alloc([128], BF16)
        w2b = w2
        rs2 = AR.alloc([8])
        rs3 = AR.alloc([8])
        junk = AR.alloc([1024], BF16)
        junk2 = AR.alloc([1024])
        accv = AR.alloc([1024])
        dg4 = [AR.alloc([GS, 128], BF16) for _ in range(2)]
        gbuf = [AR.alloc([2048], BF16) for _ in range(NB)]
        for kc in range(8):
            dma("pool", wq[:, kc, :], peer_wq[kc * 128:(kc + 1) * 128, :], [], ["wq"], "wq")
        dma("pool", kTs, keysT.rearrange("p (a b) -> p a b", b=128), [], ["kTs"], "kTs")
        dma("sp", ident, c_ident, [], ["ident"], "identD")
        dma("sp", identf, c_identf, [], ["identf"], "identfD")
        dma("sp", nfw_sb, nfw, [], ["nfw"], "nfw")
        dma("sp", nlw_sb, nlw, [], ["nlw"], "nlw")
        dma("sp", iota_sb, c_iota.rearrange("p (a b) -> p a b", b=16), [], ["iota"], "iota")
        gcnt = [0]
        dcnt = [0]
        rrb2 = [0]
        if debug:
            print('B2 arena words', AR.off)

        def nbank2():
            bk = rrb2[0] % 4
            rrb2[0] += 1
            return bk, "ps%d" % bk

        def top16(src, wk, vout, iout, ksrc, kwork, kv, ki):
            P.op("dve", lambda e: e.max(out=vout[:, 0:8], in_=src), reads=[ksrc], writes=[kv])
            P.op("dve", lambda e: e.max_index(out=iout[:, 0:8], in_max=vout[:, 0:8], in_values=src),
                 reads=[ksrc, kv], writes=[ki])
            P.op("dve", lambda e: e.match_replace(out=wk, in_to_replace=vout[:, 0:8], in_values=src,
                                                  imm_value=-1e30), reads=[ksrc, kv], writes=[kwork])
            P.op("dve", lambda e: e.max(out=vout[:, 8:16], in_=wk), reads=[kwork], writes=[kv])
            P.op("dve", lambda e: e.max_index(out=iout[:, 8:16], in_max=vout[:, 8:16], in_values=wk),
                 reads=[kwork, kv], writes=[ki])

        def front(i):
            b = i % 2
            rows = slice(i * 128, (i + 1) * 128)
            kx, khx = "x1t%d" % b, "hx%d" % b
            dma("sp", x1t[b], x1s[rows, :], [], [kx], kx)
            rs = rs2[:, 0:1]
            stt(hx, x1t[b], 1.0, x1t[b], ALU.mult, ALU.mult, [kx], ["hxf", "rs2"], accum_out=rs)
            yield
            rstd_from_ss(rs, D, "rs2")
            yield
            stt(hbf[b], x1t[b], rs, nfw_sb, ALU.mult, ALU.mult, [kx, "rs2", "nfw"], [khx])
            yield
            bk, kb = nbank2()
            for kc in range(8):
                tr(psb16(bk)[:, kc * 128:(kc + 1) * 128], hbf[b][:, kc * 128:(kc + 1) * 128], ident, [khx, "ident"], [kb])
            cp("act", hnT, psb16(bk).rearrange("p (a b) -> p a b", b=128), [kb], ["hnT"])
            for grp in range(4):
                bk, kb = nbank2()
                for j in range(4):
                    ch = 4 * grp + j
                    for kc in range(8):
                        mm(psb(bk)[:, j * 128:(j + 1) * 128], wq[:, kc, ch * 128:(ch + 1) * 128], hnT[:, kc, :],
                           kc == 0, kc == 7, ["wq", "hnT"], [kb])
                cp("act", qpT[:, 4 * grp:4 * grp + 4, :], psb(bk).rearrange("p (a b) -> p a b", b=128), [kb], ["qpT"])
            for grp in range(4):
                bk, kb = nbank2()
                for j in range(4):
                    hc = 4 * grp + j
                    mm(psb(bk)[:, j * 128:(j + 1) * 128], qpT[:, hc, :], kTs[:, hc, :], True, True, ["qpT", "kTs"], [kb])
                cp("act", s_all[:, 4 * grp:4 * grp + 4, :], psb(bk).rearrange("p (a b) -> p a b", b=128), [kb], ["scand"])
            for _sp in range(8):
                yield
            for hc in range(16):
                top16(s_all[:, hc, :], s_w[:, hc, :], vals[:, hc, :], idxu[:, hc, :], "scand", "work", "vals", "idxu")
                yield
            cp("act", i12f, idxu, ["idxu"], ["i12f"])
            vals4 = vals.rearrange("p (h c) k -> p h c k", c=2)
            i12f4 = i12f.rearrange("p (h c) k -> p h c k", c=2)
            yield
            yield
            tt("pool", cand.rearrange("p h (a b) -> p h a b", b=16),
               bc(vals4[:, :, 0, :].unsqueeze(3), [128, 8, 16, 16]),
               bc(vals4[:, :, 1, :].unsqueeze(2), [128, 8, 16, 16]), ALU.add, ["vals"], ["scand"])
            for _sp in range(4):
                yield
            for h in range(8):
                top16(cand[:, h, :], cand_w[:, h, :], sc[:, h, :], flat[:, h, :], "scand", "work", "sc", "flat")
                yield
            for which, (dstf, sh_op, sh_val) in enumerate([(e1f, ALU.logical_shift_right, 4), (e2f, ALU.bitwise_and, 15)]):
                ts("dve", fab, flat, sh_val, None, sh_op, None, ["flat"], ["fab"])
                cp("act", fabf, fab, ["fab"], ["fabf"])
                yield
                yield
                tt("dve", oh, iota_sb, bc(fabf.rearrange("p a b -> p (a b)").unsqueeze(2), [128, 128, 16]),
                   ALU.is_equal, ["iota", "fabf"], ["work"])
                yield
                oh4 = oh.rearrange("p (h k) a -> p h k a", k=16)
                tt("pool", oh4, oh4, bc(i12f4[:, :, which, :].unsqueeze(2), [128, 8, 16, 16]), ALU.mult,
                   ["work", "i12f"], ["work"])
                for _sp in range(4):
                    yield
                P.op("dve", lambda e, dstf=dstf: e.tensor_reduce(out=dstf, in_=oh, axis=AX.X, op=ALU.add),
                     reads=["work"], writes=["e12_%d" % which])
                yield
            stt(idxf, e1f, 128.0, e2f, ALU.mult, ALU.add, ["e12_0", "e12_1"], ["idxf"])
            cp("dve", idx_i[b], idxf, ["idxf"], ["idx%d" % b])
            tt("pool", es, sc, bc(sc[:, :, 0:1], [128, 8, 16]), ALU.subtract, ["sc"], ["es"])
            act(es, es, AF.Exp, ["es"], ["es"])
            yield
            yield
            P.op("dve", lambda e: e.tensor_reduce(out=ssum, in_=es, axis=AX.X, op=ALU.add), reads=["es"], writes=["ssum"])
            P.op("dve", lambda e: e.reciprocal(ssum, ssum), reads=["ssum"], writes=["ssum"])
            tt("pool", gsm[b].rearrange("p (a b) -> p a b", b=16), es, bc(ssum.unsqueeze(2), [128, 8, 16]), ALU.mult,
               ["es", "ssum"], ["gsm%d" % b])
            yield

        def back(i, tail_prev):
            b = i % 2
            kx, khx, kidx = "x1t%d" % b, "hx%d" % b, "idx%d" % b
            pa = 4 + 2 * b
            ngrp = 128 // GS
            pendq = []

            def finish(j0, bufs):
                js = slice(j0, j0 + GS)
                dk = dcnt[0] % 2
                dcnt[0] += 1
                tt("dve", w2[:, js], w_all[:, js], gsm[b][:, js], ALU.mult, ["w_all", "gsm%d" % b], ["w2"])
                tt("dve", dg4[dk], bc(ident.unsqueeze(1), [128, GS, 128]),
                   bc(w2b[:, js].unsqueeze(2), [128, GS, 128]), ALU.mult, ["ident", "w2"], ["dg%d" % dk])
                for jj, j in enumerate(range(j0, j0 + GS)):
                    bf, kgb = bufs[jj]
                    for half in range(2):
                        mm(psb(pa + half), dg4[dk][:, jj, :], gbuf[bf][:, 1024 + half * 512:1024 + (half + 1) * 512],
                           False, j == 127, ["dg%d" % dk, kgb], ["ps%d" % (pa + half)])

            for half in range(2):
                mm(psb(pa + half), identf, x1t[b][:, half * 512:(half + 1) * 512], True, False,
                   ["identf", kx], ["ps%d" % (pa + half)])
            for g_ in range(ngrp):
                j0 = g_ * GS
                bufs = []
                for j in range(j0, j0 + GS):
                    bf = gcnt[0] % NB
                    gcnt[0] += 1
                    kgb = "g%d" % bf
                    bufs.append((bf, kgb))
                    P.op("pool", lambda e, bf=bf, j=j, b=b: e.indirect_dma_start(
                        out=gbuf[bf], out_offset=None, in_=uvb,
                        in_offset=bass.IndirectOffsetOnAxis(ap=idx_i[b][:, j:j + 1], axis=0)),
                        reads=[kidx], writes=[kgb], dma=True, semkey=kgb)
                    pk = pcnt[0] % 4
                    pcnt[0] += 1
                    tt("dve", prod[pk], gbuf[bf][:, 0:1024], hbf[b], ALU.mult, [kgb, khx], ["prod%d" % pk])
                    act(junkA, prod[pk], AF.Copy, ["prod%d" % pk], ["junkA", "a_all"],
                        accum_out=a_all[b][:, j:j + 1])
                js = slice(j0, j0 + GS)
                act(w_all[:, js], a_all[b][:, js], AF.Gelu, ["a_all"], ["w_all"])
                pendq.append((j0, bufs))
                if len(pendq) > 2:
                    finish(*pendq.pop(0))
                if g_ == 2 and tail_prev is not None:
                    tail_prev()
                yield
            while pendq:
                finish(*pendq.pop(0))

        def make_tail(i):
            b = i % 2
            rows = slice(i * 128, (i + 1) * 128)
            kx = "x1t%d" % b
            pa = 4 + 2 * b

            def tail():
                for half in range(2):
                    hs_ = slice(half * 512, (half + 1) * 512)
                    cp("act", accv[:, hs_], psb(pa + half), ["ps%d" % (pa + half)], ["accv"])
                rs = rs3[:, 0:1]
                stt(junk2, accv, 1.0, accv, ALU.mult, ALU.mult, ["accv"], ["junk2", "rs3"], accum_out=rs)
                rstd_from_ss(rs, D, "rs3")
                stt(junk2, accv, rs, nlw_sb, ALU.mult, ALU.mult, ["accv", "rs3", "nlw"], ["junk2"])
                dma("sp", out[rows, :], junk2, ["junk2"], [], "junk2")
            return tail

        pcnt = [0]
        for _ in front(0):
            pass
        tail_prev = None
        for i in range(NT):
            gf = front(i + 1) if i + 1 < NT else None
            for _ in back(i, tail_prev):
                if gf is not None:
                    for _k in range(3):
                        try:
                            next(gf)
                        except StopIteration:
                            gf = None
                            break
            if gf is not None:
                for _ in gf:
                    pass
            tail_prev = make_tail(i)
        tail_prev()
        P.barrier()

    outkeys = [k for k in P.dma_cnt]
    P.emit(out_semkeys=outkeys)
    st.close()
    return nc


def host_consts():
    bf = ml_dtypes.bfloat16
    k = np.arange(128)[:, None]
    q = np.arange(128)[None, :]
    mc = np.where(k <= q, 0.0, NEG).astype(np.float32)
    mp = np.where(k > q, 0.0, NEG).astype(np.float32)
    c = {
        "c_ident": np.eye(128, dtype=np.float32).astype(bf),
        "c_identf": np.eye(128, dtype=np.float32),
        "c_maskc": np.tile(mc, (1, 4)).astype(bf),
        "c_maskp": np.tile(mp, (1, 4)).astype(bf),
        "c_U": (k <= q).astype(np.float32),
        "c_L": (k > q).astype(np.float32),
        "c_ones": np.ones((128, 128), np.float32),
        "c_iota": np.tile(np.arange(16, dtype=np.float32), (128, 128)).astype(bf),
        "c_invf": np.tile((500000.0 ** (-np.arange(0, 16, 2, dtype=np.float32) / 16.0)).astype(np.float32),
                          (128, 1)),
    }
    return c


def rep(v, n=128):
    return np.ascontiguousarray(np.broadcast_to(np.asarray(v, np.float32).reshape(1, -1), (n, v.size)))


def make_in_maps(inputs, ncores=8):
    g = lambda k: np.asarray(inputs[k])
    shared = dict(host_consts())
    shared["w_in"] = np.ascontiguousarray(g("w_in")[0])
    shared["w_attn_o"] = np.ascontiguousarray(g("w_attn_o")[0])
    shared["w_ssd_o"] = np.ascontiguousarray(g("w_ssd_o")[0])
    shared["w_out"] = np.ascontiguousarray(g("w_out")[0])
    shared["peer_wq"] = np.ascontiguousarray(g("peer_wq")[0])
    shared["keysT"] = np.ascontiguousarray(g("peer_keys")[0].transpose(3, 0, 1, 2).reshape(128, 2048))
    shared["peer_uv"] = np.ascontiguousarray(
        np.concatenate([g("peer_u")[0], g("peer_v")[0]], axis=1))
    shared["nmw"] = rep(g("norm_mix_w")[0])
    shared["nfw"] = rep(g("norm_ffn_w")[0])
    shared["nlw"] = rep(g("norm_final_w"))
    shared["snwc"] = np.ascontiguousarray(g("ssd_norm_w")[0].reshape(16, 128).T)
    shared["sinks"] = rep(g("attn_sinks")[0])
    shared["convw"] = np.ascontiguousarray(g("conv_w")[0].reshape(4, 24, 128).transpose(2, 1, 0).reshape(128, 96))
    shared["convb"] = np.ascontiguousarray(g("conv_b")[0].reshape(24, 128).T)
    shared["dtb"] = rep(g("dt_bias")[0])
    shared["alog"] = rep(g("a_log")[0])
    shared["dskip"] = rep(g("d_skip")[0])
    maps = []
    xs = g("x")
    ps = g("positions")
    for c in range(ncores):
        m = dict(shared)
        m["x"] = np.ascontiguousarray(xs[c])
        m["pos"] = np.ascontiguousarray(ps[c].reshape(32, 128).T.astype(np.int32))
        maps.append(m)
    return maps


def kernel(**inputs):
    nc = build_program(32, debug=False)
    maps = make_in_maps(inputs, 8)
    res = run_bass_kernel_spmd(nc, maps, core_ids=list(range(8)))
    return np.stack([np.asarray(r["out"]) for r in res.results], axis=0).astype(np.float32)
```

```python
import numpy as np
import ml_dtypes
from contextlib import ExitStack
import concourse.bass as bass
import concourse.mybir as mybir
from concourse.bass_utils import run_bass_kernel_spmd

F32 = mybir.dt.float32
BF16 = mybir.dt.bfloat16
I32 = mybir.dt.int32
U32 = mybir.dt.uint32
AF = mybir.ActivationFunctionType
ALU = mybir.AluOpType
AX = mybir.AxisListType

D = 1024
SEQ = 4096
NEG = -30000.0
EPS = 1e-6
IN_DIM = 8736
TMW = 5664
ENGS = ["pe", "act", "dve", "pool", "sp"]
ARENA_WORDS = 45000


class Prog:
    def __init__(self, nc):
        self.nc = nc
        self.ops = {e: [] for e in ENGS}
        self.lastw = {}
        self.readers = {}
        self.dma_cnt = {}
        self.fence = {e: None for e in ENGS}

    def op(self, eng, fn, reads=(), writes=(), dma=False, semkey=None):
        idx = len(self.ops[eng])
        raw, other = set(), set()
        for k in reads:
            raw.update(self.lastw.get(k, {}).values())
        for k in writes:
            raw.update(self.lastw.get(k, {}).values())
            other.update(self.readers.get(k, {}).values())
        deps = set()
        for d in raw | other:
            de, di = d
            drec = self.ops[de][di]
            if de == eng and not drec["dma"] and not dma:
                if eng == "pe" or d not in raw:
                    continue
            deps.add(d)
        rec = dict(fn=fn, deps=deps, dma=dma, needed=False, inc=None, fence=self.fence[eng])
        self.fence[eng] = None
        if dma:
            c = self.dma_cnt.get(semkey, 0) + 1
            self.dma_cnt[semkey] = c
            rec["semkey"] = semkey
            rec["dma_val"] = 16 * c
            tk = ("d", eng, semkey)
        else:
            tk = ("c", eng)
        self.ops[eng].append(rec)
        me = (eng, idx)
        for k in reads:
            self.readers.setdefault(k, {})[tk] = me
        for k in writes:
            self.lastw.setdefault(k, {})[tk] = me
        return me

    def barrier(self):
        snap_c = {}
        for e in ENGS:
            for i in range(len(self.ops[e]) - 1, -1, -1):
                if not self.ops[e][i]["dma"]:
                    snap_c[e] = i
                    self.ops[e][i]["needed"] = True
                    break
        snap = (snap_c, dict(self.dma_cnt))
        for e in ENGS:
            self.fence[e] = snap

    def emit(self, out_semkeys=()):
        nc = self.nc
        for e in ENGS:
            for rec in self.ops[e]:
                for (de, di) in rec["deps"]:
                    d = self.ops[de][di]
                    if not d["dma"]:
                        d["needed"] = True
        for e in ENGS:
            c = 0
            for rec in self.ops[e]:
                if rec["needed"] and not rec["dma"]:
                    c += 1
                    rec["inc"] = c
        with ExitStack() as st:
            esem = {e: st.enter_context(nc.semaphore("sem_" + e)) for e in ENGS}
            dsem = {}
            for i, k in enumerate(self.dma_cnt):
                dsem[k] = st.enter_context(nc.semaphore("dsem_%d" % i))
            block = st.enter_context(nc.Block())
            prog = self

            def body(ename):
                def f(eng):
                    waited = {}

                    def w(key, s, v):
                        if waited.get(key, 0) >= v:
                            return
                        waited[key] = v
                        eng.wait_ge(s, v)

                    for rec in prog.ops[ename]:
                        if rec["fence"] is not None:
                            sc, sd = rec["fence"]
                            for fe, fi in sc.items():
                                w(("e", fe), esem[fe], prog.ops[fe][fi]["inc"])
                            for k, cnt in sd.items():
                                w(("d", k), dsem[k], 16 * cnt)
                        for (de, di) in sorted(rec["deps"]):
                            d = prog.ops[de][di]
                            if d["dma"]:
                                w(("d", d["semkey"]), dsem[d["semkey"]], d["dma_val"])
                            else:
                                w(("e", de), esem[de], d["inc"])
                        ins = rec["fn"](eng)
                        if rec["dma"]:
                            ins.then_inc(dsem[rec["semkey"]], 16)
                        elif rec["needed"]:
                            ins.then_inc(esem[ename], 1)
                    if ename == "sp":
                        for k in out_semkeys:
                            eng.wait_ge(dsem[k], 16 * prog.dma_cnt[k])
                return f

            block.tensor(body("pe"))
            block.scalar(body("act"))
            block.vector(body("dve"))
            block.gpsimd(body("pool"))
            block.sync(body("sp"))


class Arena:
    def __init__(self, base_ap, words):
        self.base = base_ap
        self.words = words
        self.off = 0

    def reset(self):
        self.off = 0

    def alloc(self, shape, dtype=F32):
        n = int(np.prod(shape))
        if dtype == BF16:
            w = (n + 1) // 2
        else:
            w = n
        w = (w + 7) // 8 * 8
        a = self.off
        self.off += w
        assert self.off <= self.words, "arena overflow %d > %d" % (self.off, self.words)
        v = self.base[:, a:a + w]
        if dtype != F32:
            v = v.bitcast(dtype)
        v = v[:, 0:n]
        if len(shape) == 2:
            v = v.rearrange("p (a b) -> p a b", b=shape[1])
        elif len(shape) == 3:
            v = v.rearrange("p (a b c) -> p a b c", b=shape[1], c=shape[2])
        return v


def build_program(NT=32, debug=False, phases=("A", "A2", "B1a", "B1b", "B2")):
    T = NT * 128
    nc = bass.Bass("TRN2", target_bir_lowering=False)

    def din(name, shape, dt=F32):
        return nc.dram_tensor(name, list(shape), dt, kind="ExternalInput").ap()

    skind = "ExternalOutput" if debug else "Internal"

    def dscr(name, shape, dt=F32):
        return nc.dram_tensor(name, list(shape), dt, kind=skind).ap()

    x = din("x", [SEQ, D])
    pos = din("pos", [128, 32], I32)
    w_in = din("w_in", [D, IN_DIM])
    w_attn_o = din("w_attn_o", [1024, D])
    w_ssd_o = din("w_ssd_o", [2048, D])
    w_out = din("w_out", [D, D])
    peer_wq = din("peer_wq", [D, 2048])
    keysT = din("keysT", [128, 16 * 128])
    peer_uv = din("peer_uv", [16384, 2 * D])
    nmw = din("nmw", [128, D])
    nfw = din("nfw", [128, D])
    nlw = din("nlw", [128, D])
    snwc = din("snwc", [128, 16])
    sinks = din("sinks", [128, 16])
    convw = din("convw", [128, 24 * 4])
    convb = din("convb", [128, 24])
    dtb = din("dtb", [128, 32])
    alog = din("alog", [128, 32])
    dskip = din("dskip", [128, 32])
    c_ident = din("c_ident", [128, 128], BF16)
    c_identf = din("c_identf", [128, 128])
    c_maskc = din("c_maskc", [128, 512], BF16)
    c_maskp = din("c_maskp", [128, 512], BF16)
    c_U = din("c_U", [128, 128])
    c_L = din("c_L", [128, 128])
    c_ones = din("c_ones", [128, 128])
    c_iota = din("c_iota", [128, 2048], BF16)
    c_invf = din("c_invf", [128, 8])
    out = nc.dram_tensor("out", [SEQ, D], F32, kind="ExternalOutput").ap()
    pt = dscr("pt", [SEQ, TMW])
    xbcT = dscr("xbcT", [3072, SEQ])
    xc = dscr("xc", [3072, SEQ])
    ma = dscr("ma", [SEQ, D])
    x1s = dscr("x1s", [SEQ, D])
    uvb = nc.dram_tensor("uvb", [16384, 2 * D], BF16, kind="Internal").ap()

    st = ExitStack()
    arena_t = st.enter_context(nc.sbuf_tensor("arena", [128, ARENA_WORDS], F32))
    AR = Arena(arena_t[:], ARENA_WORDS)
    PS = [st.enter_context(nc.psum_tensor("ps%d" % i, [128, 512], F32)) for i in range(8)]
    P = Prog(nc)

    def dma(q, out_ap, in_ap, reads, writes, semkey):
        P.op(q, lambda e: e.dma_start(out=out_ap, in_=in_ap), reads=reads, writes=writes,
             dma=True, semkey=semkey)

    def mm(out_ap, lhsT, rhs, start, stop, reads, writes, skip=False):
        P.op("pe", lambda e: e.matmul(out_ap, lhsT, rhs, start=start, stop=stop,
                                      skip_group_check=skip), reads=reads, writes=writes)

    def tr(out_ap, in_ap, ident, reads, writes):
        P.op("pe", lambda e: e.transpose(out_ap, in_ap, ident), reads=reads, writes=writes)

    def act(out_ap, in_ap, func, reads, writes, bias=None, scale=None, accum_out=None):
        kw = {}
        if bias is not None:
            kw["bias"] = bias
        if scale is not None:
            kw["scale"] = scale
        if accum_out is not None:
            kw["accum_out"] = accum_out
        P.op("act", lambda e: e.activation(out_ap, in_ap, func, **kw), reads=reads, writes=writes)

    def tt(eng, out_ap, in0, in1, op, reads, writes):
        P.op(eng, lambda e: e.tensor_tensor(out_ap, in0, in1, op), reads=reads, writes=writes)

    def ts(eng, out_ap, in0, s1, s2, op0, op1, reads, writes, accum_out=None):
        if op1 is None:
            P.op(eng, lambda e: e.tensor_scalar(out_ap, in0, s1, None, op0), reads=reads, writes=writes)
        else:
            P.op(eng, lambda e: e.tensor_scalar(out_ap, in0, s1, s2, op0, op1, accum_out=accum_out),
                 reads=reads, writes=writes)

    def stt(out_ap, in0, scalar, in1, op0, op1, reads, writes, accum_out=None):
        P.op("dve", lambda e: e.scalar_tensor_tensor(out_ap, in0, scalar, in1, op0, op1,
                                                     accum_out=accum_out), reads=reads, writes=writes)

    def cp(eng, out_ap, in_ap, reads, writes):
        if eng == "act":
            P.op("act", lambda e: e.copy(out_ap, in_ap), reads=reads, writes=writes)
        else:
            P.op(eng, lambda e: e.tensor_copy(out_ap, in_ap), reads=reads, writes=writes)

    def rstd_from_ss(rs, n, key):
        ts("dve", rs, rs, 1.0 / n, EPS, ALU.mult, ALU.add, [key], [key])
        act(rs, rs, AF.Sqrt, [key], [key])
        P.op("dve", lambda e: e.reciprocal(rs, rs), reads=[key], writes=[key])

    def psb(i):
        return PS[i][:]

    def psb16(i):
        return PS[i][:].bitcast(BF16)


    CVW = 4096
    cvreg = arena_t[:, ARENA_WORDS - CVW:ARENA_WORDS].bitcast(BF16)
    cvb = [cvreg[:, s_ * 2048:(s_ + 1) * 2048] for s_ in range(4)]
    cv_state = [0, 0]
    NCH = 128

    def conv_steps(k):
        for _ in range(k):
            c = cv_state[0]
            if c < NCH:
                sidx = c % 4
                dma("pool", cvb[sidx], peer_uv[c * 128:(c + 1) * 128, :], [], ["cv%d" % sidx], "cvl%d" % sidx)
                cv_state[0] += 1
            if cv_state[0] - cv_state[1] > 2 or (cv_state[0] == NCH and cv_state[1] < NCH):
                c2 = cv_state[1]
                sidx = c2 % 4
                dma("pool", uvb[c2 * 128:(c2 + 1) * 128, :], cvb[sidx], ["cv%d" % sidx], [], "cvs%d" % sidx)
                cv_state[1] += 1

    def conv_flush():
        while cv_state[1] < NCH:
            conv_steps(1)

    if "A" in phases:
        AR.reset()
        wtm = AR.alloc([8, TMW], BF16)
        wfm = AR.alloc([8, 3072], BF16)
        nw = AR.alloc([D])
        ident = AR.alloc([128], BF16)
        xt = [AR.alloc([D]) for _ in range(2)]
        hb = [AR.alloc([D], BF16) for _ in range(2)]
        hT = [AR.alloc([8, 128], BF16) for _ in range(2)]
        stg = [AR.alloc([512]) for _ in range(4)]
        ssq = [AR.alloc([8]) for _ in range(2)]
        dma("sp", nw, nmw, [], ["nw"], "nw")
        dma("sp", ident, c_ident, [], ["ident"], "ident")
        pieces_tm = [(0, 1792, 0), (1792, 3584, 1792), (6656, 7696, 3584), (7696, 8736, 4624)]
        pieces_fm = [(3584, 5120, 0), (5120, 6656, 1536)]
        for kc in range(8):
            for (c0, c1, d0) in pieces_tm:
                dma("pool", wtm[:, kc, d0:d0 + (c1 - c0)], w_in[kc * 128:(kc + 1) * 128, c0:c1],
                    [], ["wtm"], "wtm")
            for (c0, c1, d0) in pieces_fm:
                dma("pool", wfm[:, kc, d0:d0 + (c1 - c0)], w_in[kc * 128:(kc + 1) * 128, c0:c1],
                    [], ["wfm"], "wfm")
        bank_rr = [0]
        stg_rr = [0]
        for i in range(NT):
            b = i % 2
            kx, kh, khT, ks = "xt%d" % b, "hb%d" % b, "hT%d" % b, "ssq%d" % b
            dma("sp", xt[b], x[i * 128:(i + 1) * 128, :], [], [kx], kx)
            rs = ssq[b][:, 0:1]
            stt(hb[b], xt[b], 1.0, xt[b], ALU.mult, ALU.mult, [kx], [kh, ks], accum_out=rs)
            rstd_from_ss(rs, D, ks)
            stt(hb[b], xt[b], rs, nw, ALU.mult, ALU.mult, [kx, ks, "nw"], [kh])
            pst = "psT%d" % b
            for kc in range(8):
                tr(psb16(b)[:, kc * 128:(kc + 1) * 128], hb[b][:, kc * 128:(kc + 1) * 128], ident,
                   [kh, "ident"], [pst])
            cp("act", hT[b], psb16(b).rearrange("p (a b) -> p a b", b=128), [pst], [khT])
            ngrp = (TMW + 511) // 512
            for cg in range(ngrp):
                c0 = cg * 512
                cw = min(512, TMW - c0)
                bk = 2 + bank_rr[0] % 6
                bank_rr[0] += 1
                kb = "psb%d" % bk
                for kc in range(8):
                    mm(psb(bk)[:, 0:cw], hT[b][:, kc, :], wtm[:, kc, c0:c0 + cw], kc == 0, kc == 7,
                       [khT, "wtm"], [kb])
                s = stg_rr[0] % 4
                stg_rr[0] += 1
                ksg = "stg%d" % s
                cp("act" if cg % 2 == 0 else "dve", stg[s][:, 0:cw], psb(bk)[:, 0:cw], [kb], [ksg])
                dma("sp", pt[i * 128:(i + 1) * 128, c0:c0 + cw], stg[s][:, 0:cw], [ksg], [], ksg)
            for grp in range(6):
                bk = 2 + bank_rr[0] % 6
                bank_rr[0] += 1
                kb = "psb%d" % bk
                for j in range(4):
                    ch = grp * 4 + j
                    for kc in range(8):
                        mm(psb(bk)[:, j * 128:(j + 1) * 128], wfm[:, kc, ch * 128:(ch + 1) * 128],
                           hT[b][:, kc, :], kc == 0, kc == 7, [khT, "wfm"], [kb])
                s = stg_rr[0] % 4
                stg_rr[0] += 1
                ksg = "stg%d" % s
                cp("act" if grp % 2 == 0 else "dve", stg[s], psb(bk), [kb], [ksg])
                dst = xbcT[grp * 512:(grp + 1) * 512, i * 128:(i + 1) * 128].rearrange(
                    "(j p) t -> p j t", p=128)
                dma("sp", dst, stg[s].rearrange("p (j t) -> p j t", t=128), [ksg], [], ksg)
        P.barrier()

    if "A2" in phases:
        AR.reset()
        cw_sb = AR.alloc([24, 4])
        cb_sb = AR.alloc([24])
        u = [AR.alloc([T + 8]) for _ in range(2)]
        acc = [AR.alloc([T]) for _ in range(2)]
        dma("sp", cw_sb, convw.rearrange("p (c j) -> p c j", j=4), [], ["cw"], "cw")
        dma("sp", cb_sb, convb, [], ["cb"], "cb")
        for b in range(2):
            P.op("dve", lambda e, b=b: e.memset(u[b][:, 0:3], 0.0), writes=["u%d" % b])
        for ch in range(24):
            b = ch % 2
            ku, ka = "u%d" % b, "acc%d" % b
            dma("sp", u[b][:, 3:3 + T], xbcT[ch * 128:(ch + 1) * 128, 0:T], [], [ku], ku)
            ts("dve", acc[b], u[b][:, 0:T], cw_sb[:, ch, 0:1], None, ALU.mult, None, [ku, "cw"], [ka])
            for j in range(1, 4):
                stt(acc[b], u[b][:, j:j + T], cw_sb[:, ch, j:j + 1], acc[b], ALU.mult, ALU.add,
                    [ku, "cw", ka], [ka])
            act(acc[b], acc[b], AF.Silu, [ka, "cb"], [ka], bias=cb_sb[:, ch:ch + 1])
            dma("sp", xc[ch * 128:(ch + 1) * 128, 0:T], acc[b], [ka], [], ka)
            if "B2" in phases:
                conv_steps(2)
        assert AR.off <= ARENA_WORDS - CVW
        P.barrier()


    def run_pipelined(make_gen, n_tiles, start_every):
        active = []
        nxt = 0
        rnd = 0
        while nxt < n_tiles or active:
            if nxt < n_tiles and rnd % start_every == 0:
                active.append(make_gen(nxt))
                nxt += 1
            still = []
            for g_ in active:
                try:
                    next(g_)
                    still.append(g_)
                except StopIteration:
                    pass
            active = still
            rnd += 1


    TWO_PI = 2.0 * np.pi

    def bc(ap, shape):
        return ap.to_broadcast(list(shape))

    if "B1a" in phases:
        AR.reset()
        wao = AR.alloc([8, 1024], BF16)
        ident = AR.alloc([128], BF16)
        maskc = AR.alloc([512], BF16)
        maskp = AR.alloc([512], BF16)
        posi = AR.alloc([32], I32)
        posf = AR.alloc([32])
        invf = AR.alloc([8])
        ang = AR.alloc([32, 8])
        cs = AR.alloc([32, 8])
        sn = AR.alloc([32, 8])
        ra = AR.alloc([32, 8])
        rb_ = AR.alloc([32, 8])
        rc = AR.alloc([32, 8])
        rki = AR.alloc([32, 8], I32)
        esink = AR.alloc([16])
        qkv = [AR.alloc([1536]) for _ in range(2)]
        gt = [AR.alloc([1024]) for _ in range(2)]
        qk_bf = [AR.alloc([20, 64], BF16) for _ in range(2)]
        kd = [AR.alloc([4, 2, 128], BF16) for _ in range(2)]
        rt = [[AR.alloc([20, 8]) for _ in range(4)] for _ in range(2)]
        qT = [AR.alloc([8, 128], BF16) for _ in range(2)]
        kT = [AR.alloc([8, 128], BF16) for _ in range(3)]
        v1 = [AR.alloc([4, 66], BF16) for _ in range(3)]
        pTc = [[AR.alloc([512], BF16) for _ in range(2)] for _ in range(2)]
        pTp = [[AR.alloc([512], BF16) for _ in range(2)] for _ in range(2)]
        den = [[AR.alloc([4]) for _ in range(2)] for _ in range(2)]
        attn = [AR.alloc([16, 64], BF16) for _ in range(2)]
        attnT = [AR.alloc([8, 128], BF16) for _ in range(2)]
        gsig = [AR.alloc([1024]) for _ in range(2)]
        mres = [AR.alloc([1024]) for _ in range(2)]
        for kc in range(8):
            dma("pool", wao[:, kc, :], w_attn_o[kc * 128:(kc + 1) * 128, :], [], ["wao"], "wao")
        dma("sp", ident, c_ident, [], ["ident"], "identB")
        dma("sp", maskc, c_maskc, [], ["maskc"], "maskc")
        dma("sp", maskp, c_maskp, [], ["maskp"], "maskp")
        dma("sp", posi, pos, [], ["posi"], "posi")
        dma("sp", invf, c_invf, [], ["invf"], "invf")
        dma("sp", esink, sinks, [], ["esink"], "esink")
        act(esink, esink, AF.Exp, ["esink"], ["esink"])
        cp("dve", posf, posi, ["posi"], ["posf"])
        tt("dve", ang, bc(posf.unsqueeze(2), [128, 32, 8]), bc(invf.unsqueeze(1), [128, 32, 8]), ALU.mult,
           ["posf", "invf"], ["ang"])

        def sin_of(dst, dkey, shift):
            ts("dve", ra, ang, shift, 1.0 / TWO_PI, ALU.add, ALU.mult, ["ang"], ["ra"])
            cp("dve", rki, ra, ["ra"], ["rki"])
            cp("dve", rb_, rki, ["rki"], ["rb"])
            ts("dve", ra, ang, shift, None, ALU.add, None, ["ang"], ["ra"])
            stt(ra, rb_, -TWO_PI, ra, ALU.mult, ALU.add, ["rb", "ra"], ["ra"])
            ts("dve", rb_, ra, float(np.pi), None, ALU.is_gt, None, ["ra"], ["rb"])
            ts("dve", rc, ra, float(-np.pi), None, ALU.is_lt, None, ["ra"], ["rc"])
            tt("dve", rc, rc, rb_, ALU.subtract, ["rb", "rc"], ["rc"])
            stt(ra, rc, TWO_PI, ra, ALU.mult, ALU.add, ["rc", "ra"], ["ra"])
            ts("dve", ra, ra, float(np.pi), float(-np.pi), ALU.min, ALU.max, ["ra"], ["ra"])
            act(dst, ra, AF.Sin, ["ra"], [dkey])

        sin_of(sn, "sn", 0.0)
        sin_of(cs, "cs", float(np.pi / 2))
        for b in range(3):
            P.op("dve", lambda e, b=b: e.memset(v1[b][:, :, 64:66], 1.0), writes=["v1_%d" % b])
        for b in range(2):
            P.op("pool", lambda e, b=b: e.memset(kd[b], 0.0), writes=["kd%d" % b])

        def b1a_tile(i):
            b = i % 2
            b3 = i % 3
            pb3 = (i - 1) % 3
            S = lambda n: "%s_%d" % (n, b)
            kq, kg = "qkv%d" % b, "gt%d" % b
            rows = slice(i * 128, (i + 1) * 128)
            dma("sp", qkv[b], pt[rows, 0:1536], [], [kq], kq)
            dma("sp", gt[b], pt[rows, 3616:4640], [], [kg], kg)
            qk = qkv[b][:, 0:1280].rearrange("p (h d) -> p h d", d=64)
            x1 = qk[:, :, 0:8]
            x2 = qk[:, :, 8:16]
            cb_ = bc(cs[:, i:i + 1, :], [128, 20, 8])
            sb_ = bc(sn[:, i:i + 1, :], [128, 20, 8])
            r_ = rt[b]
            qb = qk_bf[b]
            tt("dve", r_[0], x1, cb_, ALU.mult, [kq, "cs"], [S("rt0")])
            tt("dve", r_[1], x2, sb_, ALU.mult, [kq, "sn"], [S("rt1")])
            tt("dve", qb[:, :, 0:8], r_[0], r_[1], ALU.subtract, [S("rt0"), S("rt1")], [S("qkbf")])
            tt("dve", r_[2], x2, cb_, ALU.mult, [kq, "cs"], [S("rt2")])
            tt("dve", r_[3], x1, sb_, ALU.mult, [kq, "sn"], [S("rt3")])
            tt("dve", qb[:, :, 8:16], r_[2], r_[3], ALU.add, [S("rt2"), S("rt3")], [S("qkbf")])
            cp("pool", qb[:, :, 16:64], qk[:, :, 16:64], [kq], [S("qkbf2")])
            cp("pool", kd[b][:, :, 0, 0:64], qb[:, 16:20, :], [S("qkbf"), S("qkbf2")], ["kd%d" % b])
            cp("pool", kd[b][:, :, 1, 64:128], qb[:, 16:20, :], [S("qkbf"), S("qkbf2")], ["kd%d" % b])
            cp("act", v1[b3][:, :, 0:64], qkv[b][:, 1280:1536].rearrange("p (h d) -> p h d", d=64),
               [kq], ["v1_%d" % b3])
            kpsT = "ps%d" % b
            for j in range(8):
                tr(psb16(b)[:, j * 128:(j + 1) * 128],
                   qb[:, 2 * j:2 * j + 2, :].rearrange("p a b -> p (a b)"), ident,
                   [S("qkbf"), S("qkbf2"), "ident"], [kpsT])
            cp("act", qT[b], psb16(b).rearrange("p (a b) -> p a b", b=128), [kpsT], [S("qT")])
            for h2 in range(8):
                tr(psb16(b)[:, h2 * 128:(h2 + 1) * 128], kd[b][:, h2 // 2, h2 % 2, :], ident,
                   ["kd%d" % b, "ident"], [kpsT])
            cp("dve", kT[b3], psb16(b).rearrange("p (a b) -> p a b", b=128), [kpsT], ["kT%d" % b3])
            yield
            for h in range(4):
                hp = h % 2
                bc_, bp_, bo_ = 2 + hp * 2, 3 + hp * 2, 6 + hp
                kbc, kbp, kbo = "ps%d" % bc_, "ps%d" % bp_, "ps%d" % bo_
                pc, pp = pTc[b][hp], pTp[b][hp]
                kpc, kpp = "pTc%d_%d" % (b, hp), "pTp%d_%d" % (b, hp)
                blocks = [(bc_, kbc, maskc, "maskc", kT[b3], "kT%d" % b3, pc, kpc)]
                if i > 0:
                    blocks.append((bp_, kbp, maskp, "maskp", kT[pb3], "kT%d" % pb3, pp, kpp))
                for (bk, kb, msk, kmsk, kTt, kkT, pT_, kpT) in blocks:
                    for g in range(4):
                        half = g % 2
                        mm(psb(bk)[:, g * 128:(g + 1) * 128], ident, msk[:, 0:128], True, False,
                           ["ident", kmsk], [kb])
                        mm(psb(bk)[:, g * 128:(g + 1) * 128], kTt[:, 2 * h + half, :],
                           qT[b][:, 2 * h + g // 2, :], False, True,
                           [kkT, S("qT")], [kb])
                    act(pT_, psb(bk), AF.Exp, [kb], [kpT], scale=0.125)
                for g in range(4):
                    o_ap = psb(bo_)[:, g * 65:(g + 1) * 65]
                    if i > 0:
                        mm(o_ap, pp[:, g * 128:(g + 1) * 128], v1[pb3][:, h, 0:65], True, False,
                           [kpp, "v1_%d" % pb3], [kbo])
                        mm(o_ap, pc[:, g * 128:(g + 1) * 128], v1[b3][:, h, 0:65], False, True,
                           [kpc, "v1_%d" % b3], [kbo])
                    else:
                        mm(o_ap, pc[:, g * 128:(g + 1) * 128], v1[b3][:, h, 0:65], True, True,
                           [kpc, "v1_%d" % b3], [kbo])
                ov = psb(bo_)[:, 0:260].rearrange("p (g d) -> p g d", d=65)
                dn = den[b][hp]
                kden = "den%d_%d" % (b, hp)
                tt("dve", dn, ov[:, :, 64], esink[:, 4 * h:4 * h + 4], ALU.add, [kbo, "esink"], [kden])
                P.op("dve", lambda e, dn=dn: e.reciprocal(dn, dn), reads=[kden], writes=[kden])
                tt("dve", attn[b][:, 4 * h:4 * h + 4, :], ov[:, :, 0:64], bc(dn.unsqueeze(2), [128, 4, 64]),
                   ALU.mult, [kbo, kden], [S("attn")])
                yield
            for j in range(8):
                tr(psb16(b)[:, j * 128:(j + 1) * 128],
                   attn[b][:, 2 * j:2 * j + 2, :].rearrange("p a b -> p (a b)"), ident, [S("attn"), "ident"], [kpsT])
            cp("act", attnT[b], psb16(b).rearrange("p (a b) -> p a b", b=128), [kpsT], [S("attnT")])
            act(gsig[b], gt[b], AF.Sigmoid, [kg], [S("gsig")])
            for cg in range(2):
                bk = 2 + cg + 2 * b
                for kc in range(8):
                    mm(psb(bk), attnT[b][:, kc, :], wao[:, kc, cg * 512:(cg + 1) * 512], kc == 0, kc == 7,
                       [S("attnT"), "wao"], ["ps%d" % bk])
                tt("dve", mres[b][:, cg * 512:(cg + 1) * 512], psb(bk), gsig[b][:, cg * 512:(cg + 1) * 512],
                   ALU.mult, ["ps%d" % bk, S("gsig")], ["mres%d" % b])
            dma("sp", ma[rows, :], mres[b], ["mres%d" % b], [], "mres%d" % b)
            if "B2" in phases:
                conv_steps(3)
            yield

        run_pipelined(b1a_tile, NT, 3)
        if "B2" in phases:
            conv_flush()
        assert AR.off <= ARENA_WORDS - CVW
        P.barrier()

    if "B1b" in phases:
        AR.reset()
        wso = AR.alloc([16, 1024], BF16)
        wo = AR.alloc([8, 1024], BF16)
        ident = AR.alloc([128], BF16)
        identf = AR.alloc([128])
        maskc = AR.alloc([512], BF16)
        Um = AR.alloc([128])
        Lm = AR.alloc([128])
        onesm = AR.alloc([128])
        snw_sb = AR.alloc([16])
        dtb_sb = AR.alloc([32])
        A_sb = AR.alloc([32])
        dsk_sb = AR.alloc([32])
        one_c = AR.alloc([8])
        stateT = AR.alloc([4, 512])
        stateT_bf = AR.alloc([4, 512], BF16)
        zt = [AR.alloc([2048]) for _ in range(2)]
        dtr = [AR.alloc([32]) for _ in range(2)]
        g2 = [AR.alloc([1024]) for _ in range(2)]
        xct = [AR.alloc([24, 128]) for _ in range(2)]
        mat = [AR.alloc([1024]) for _ in range(2)]
        xt = [AR.alloc([1024]) for _ in range(2)]
        dtt = [AR.alloc([32]) for _ in range(2)]
        dA = [AR.alloc([32]) for _ in range(2)]
        acum_sb = [AR.alloc([32]) for _ in range(2)]
        ea = [AR.alloc([32]) for _ in range(2)]
        wd = [AR.alloc([32]) for _ in range(2)]
        etot = [AR.alloc([32]) for _ in range(2)]
        wdt = [AR.alloc([32]) for _ in range(2)]
        BCbf = [AR.alloc([8, 128], BF16) for _ in range(2)]
        B_tm = [AR.alloc([4, 128], BF16) for _ in range(2)]
        ynT = [AR.alloc([16, 128], BF16) for _ in range(2)]
        xdt = AR.alloc([8, 64], BF16)
        xdtw = AR.alloc([8, 64], BF16)
        xsD = AR.alloc([8, 64], BF16)
        cbT = AR.alloc([128])
        Rm = [AR.alloc([4, 128]) for _ in range(2)]
        decT = [AR.alloc([4, 128]) for _ in range(2)]
        MT = [AR.alloc([4, 128], BF16) for _ in range(2)]
        ytmp = AR.alloc([8, 64])
        yg = AR.alloc([8, 64])
        ssg = AR.alloc([8])
        yn_g = AR.alloc([512], BF16)
        mtmp = AR.alloc([1024])
        mbf = AR.alloc([1024], BF16)
        mT = AR.alloc([8, 128], BF16)
        if debug:
            print("B1b arena words", AR.off)
        dma("sp", snw_sb, snwc, [], ["snw"], "snw")
        for kc in range(16):
            sb = kc % 2
            stg_ = zt[sb][:, 0:1024]
            dma("sp", stg_, w_ssd_o[kc * 128:(kc + 1) * 128, :], [], ["zt%d" % sb], "zt%d" % sb)
            ts("dve", wso[:, kc, :], stg_, snw_sb[:, kc:kc + 1], None, ALU.mult, None,
               ["zt%d" % sb, "snw"], ["wso"])
        for kc in range(8):
            dma("pool", wo[:, kc, :], w_out[kc * 128:(kc + 1) * 128, :], [], ["wo"], "wo")
        dma("sp", ident, c_ident, [], ["ident"], "identC")
        dma("sp", identf, c_identf, [], ["identf"], "identf")
        dma("sp", maskc, c_maskc, [], ["maskc"], "maskcC")
        dma("sp", Um, c_U, [], ["Um"], "Um")
        dma("sp", Lm, c_L, [], ["Lm"], "Lm")
        dma("sp", onesm, c_ones, [], ["onesm"], "onesm")
        dma("sp", dtb_sb, dtb, [], ["dtb"], "dtb")
        dma("sp", A_sb, alog, [], ["A"], "A")
        dma("sp", dsk_sb, dskip, [], ["dsk"], "dsk")
        act(A_sb, A_sb, AF.Exp, ["A"], ["A"])
        ts("dve", A_sb, A_sb, -1.0, None, ALU.mult, None, ["A"], ["A"])
        P.op("dve", lambda e: e.memset(one_c, 1.0), writes=["one_c"])
        P.op("dve", lambda e: e.memset(stateT, 0.0), writes=["stateT"])
        P.op("dve", lambda e: e.memset(stateT_bf, 0.0), writes=["stateTbf"])
        rrb = [0]

        def nbank():
            bk = rrb[0] % 8
            rrb[0] += 1
            return bk, "ps%d" % bk

        def b1b_tile(i):
            b = i % 2
            S = lambda n: "%s_%d" % (n, b)
            rows = slice(i * 128, (i + 1) * 128)
            kzt, kdtr, kg2, kxct, kmat, kxt = ("zt%d" % b, "dtr%d" % b, "g2_%d" % b, "xct%d" % b,
                                               "mat%d" % b, "xtB%d" % b)
            dma("sp", dtr[b], pt[rows, 3584:3616], [], [kdtr], kdtr)
            dma("sp", xct[b], xc[:, i * 128:(i + 1) * 128].rearrange("(c p) t -> p c t", p=128), [], [kxct], kxct)
            dma("sp", zt[b], pt[rows, 1536:3584], [], [kzt], kzt)
            dma("sp", g2[b], pt[rows, 4640:5664], [], [kg2], kg2)
            dma("sp", mat[b], ma[rows, :], [], [kmat], kmat)
            dma("sp", xt[b], x[rows, :], [], [kxt], kxt)
            tt("dve", dtt[b], dtr[b], dtb_sb, ALU.add, [kdtr, "dtb"], [S("dtt")])
            act(dtt[b], dtt[b], AF.Exp, [S("dtt")], [S("dtt")])
            act(dtt[b], dtt[b], AF.Ln, [S("dtt"), "one_c"], [S("dtt")], bias=one_c[:, 0:1])
            tt("dve", dA[b], dtt[b], A_sb, ALU.mult, [S("dtt"), "A"], [S("dA")])
            bk, kb = nbank()
            mm(psb(bk)[:, 0:32], Um, dA[b], True, True, ["Um", S("dA")], [kb])
            mm(psb(bk)[:, 32:64], onesm, dA[b], True, True, ["onesm", S("dA")], [kb])
            cp("dve", acum_sb[b], psb(bk)[:, 0:32], [kb], [S("acum")])
            act(ea[b], psb(bk)[:, 0:32], AF.Exp, [kb], [S("ea")])
            act(etot[b], psb(bk)[:, 32:64], AF.Exp, [kb], [S("etot")])
            tt("dve", wd[b], psb(bk)[:, 32:64], acum_sb[b], ALU.subtract, [kb, S("acum")], [S("wd")])
            act(wd[b], wd[b], AF.Exp, [S("wd")], [S("wd")])
            tt("dve", wdt[b], wd[b], dtt[b], ALU.mult, [S("wd"), S("dtt")], [S("wdt")])
            cp("act", BCbf[b], xct[b][:, 16:24, :], [kxct], [S("BCbf")])
            bk, kb = nbank()
            for g in range(4):
                tr(psb16(bk)[:, g * 128:(g + 1) * 128], BCbf[b][:, g, :], ident, [S("BCbf"), "ident"], [kb])
            cp("act", B_tm[b], psb16(bk)[:, 0:512].rearrange("p (a b) -> p a b", b=128), [kb], [S("B_tm")])
            yield
            for g in range(4):
                hs = slice(8 * g, 8 * g + 8)
                bkx, kx = nbank()
                for j in range(4):
                    tr(psb(bkx)[:, j * 128:(j + 1) * 128], xct[b][:, 4 * g + j, :], identf, [kxct, "identf"], [kx])
                xv = psb(bkx).rearrange("p (h d) -> p h d", d=64)
                tt("dve", xdt, xv, bc(dtt[b][:, hs].unsqueeze(2), [128, 8, 64]), ALU.mult, [kx, S("dtt")], ["xdt"])
                tt("dve", xdtw, xv, bc(wdt[b][:, hs].unsqueeze(2), [128, 8, 64]), ALU.mult, [kx, S("wdt")], ["xdtw"])
                tt("dve", xsD, xv, bc(dsk_sb[:, hs].unsqueeze(2), [128, 8, 64]), ALU.mult, [kx, "dsk"], ["xsD"])
                bk, kb = nbank()
                mm(psb(bk)[:, 0:128], BCbf[b][:, g, :], BCbf[b][:, 4 + g, :], True, True, [S("BCbf")], [kb])
                cp("act", cbT, psb(bk)[:, 0:128], [kb], ["cbT"])
                bky, kby = nbank()
                for blk in range(2):
                    h0 = 8 * g + 4 * blk
                    rb2 = blk
                    tt("pool", Rm[rb2], bc(Um.unsqueeze(1), [128, 4, 128]),
                       bc(dA[b][:, h0:h0 + 4].unsqueeze(2), [128, 4, 128]), ALU.mult, ["Um", S("dA")], ["Rm%d" % rb2])
                    bkd, kbd = nbank()
                    mm(psb(bkd), ident, maskc, True, False, ["ident", "maskc"], [kbd])
                    mm(psb(bkd), Lm, Rm[rb2].rearrange("p a b -> p (a b)"), False, True, ["Lm", "Rm%d" % rb2], [kbd])
                    act(decT[rb2], psb(bkd).rearrange("p (a b) -> p a b", b=128), AF.Exp, [kbd], ["decT%d" % rb2])
                    tt("dve", MT[rb2], decT[rb2], bc(cbT.unsqueeze(1), [128, 4, 128]), ALU.mult,
                       ["decT%d" % rb2, "cbT"], ["MT%d" % rb2])
                    for j in range(4):
                        hh = 4 * blk + j
                        mm(psb(bky)[:, hh * 64:(hh + 1) * 64], ident, xsD[:, hh, :], True, False,
                           ["ident", "xsD"], [kby])
                        mm(psb(bky)[:, hh * 64:(hh + 1) * 64], MT[rb2][:, j, :], xdt[:, hh, :], False, True,
                           ["MT%d" % rb2, "xdt"], [kby])
                bki, kbi = nbank()
                mm(psb(bki), BCbf[b][:, 4 + g, :], stateT_bf[:, g, :], True, True, [S("BCbf"), "stateTbf"], [kbi])
                tt("dve", ytmp, psb(bki).rearrange("p (h d) -> p h d", d=64),
                   bc(ea[b][:, hs].unsqueeze(2), [128, 8, 64]), ALU.mult, [kbi, S("ea")], ["ytmp"])
                tt("dve", yg, psb(bky).rearrange("p (h d) -> p h d", d=64), ytmp, ALU.add, [kby, "ytmp"], ["yg"])
                bks, kbs = nbank()
                mm(psb(bks), B_tm[b][:, g, :], xdtw.rearrange("p a b -> p (a b)"), True, True,
                   [S("B_tm"), "xdtw"], [kbs])
                sv = stateT[:, g, :].rearrange("p (h d) -> p h d", d=64)
                tt("pool", sv, sv, bc(etot[b][:, hs].unsqueeze(2), [128, 8, 64]), ALU.mult, ["stateT", S("etot")], ["stateT"])
                tt("dve", stateT[:, g, :], stateT[:, g, :], psb(bks), ALU.add, ["stateT", kbs], ["stateT"])
                cp("act", stateT_bf[:, g, :], stateT[:, g, :], ["stateT"], ["stateTbf"])
                zg = zt[b][:, g * 512:(g + 1) * 512]
                act(zg, zg, AF.Silu, [kzt], [kzt])
                ygf = yg.rearrange("p a b -> p (a b)")
                tt("dve", ygf, ygf, zg, ALU.mult, ["yg", kzt], ["yg"])
                rs = ssg[:, g:g + 1]
                stt(ytmp.rearrange("p a b -> p (a b)"), ygf, 1.0, ygf, ALU.mult, ALU.mult, ["yg"], ["ytmp", "ssg"], accum_out=rs)
                rstd_from_ss(rs, 512, "ssg")
                ts("dve", yn_g, ygf, rs, None, ALU.mult, None, ["yg", "ssg"], ["yn_g"])
                bk, kb = nbank()
                for j in range(4):
                    tr(psb16(bk)[:, j * 128:(j + 1) * 128], yn_g[:, j * 128:(j + 1) * 128], ident, ["yn_g", "ident"], [kb])
                cp("act", ynT[b][:, 4 * g:4 * g + 4, :], psb16(bk)[:, 0:512].rearrange("p (a b) -> p a b", b=128), [kb], [S("ynT")])
                yield
            act(g2[b], g2[b], AF.Sigmoid, [kg2], [kg2])
            for cg in range(2):
                bk, kb = nbank()
                cs_ = slice(cg * 512, (cg + 1) * 512)
                for kc in range(16):
                    mm(psb(bk), ynT[b][:, kc, :], wso[:, kc, cs_], kc == 0, kc == 15, [S("ynT"), "wso"], [kb])
                tt("dve", mtmp[:, cs_], psb(bk), g2[b][:, cs_], ALU.mult, [kb, kg2], ["mtmp"])
                tt("dve", mbf[:, cs_], mtmp[:, cs_], mat[b][:, cs_], ALU.add, ["mtmp", kmat], ["mbf"])
            bk, kb = nbank()
            for j in range(8):
                tr(psb16(bk)[:, j * 128:(j + 1) * 128], mbf[:, j * 128:(j + 1) * 128], ident, ["mbf", "ident"], [kb])
            cp("act", mT, psb16(bk).rearrange("p (a b) -> p a b", b=128), [kb], ["mT"])
            for cg in range(2):
                bk, kb = nbank()
                cs_ = slice(cg * 512, (cg + 1) * 512)
                for kc in range(8):
                    mm(psb(bk), mT[:, kc, :], wo[:, kc, cs_], kc == 0, kc == 7, ["mT", "wo"], [kb])
                tt("dve", mtmp[:, cs_], psb(bk), xt[b][:, cs_], ALU.add, [kb, kxt], ["mtmp"])
            dma("sp", x1s[rows, :], mtmp, ["mtmp"], [], "mtmp")
            yield

        run_pipelined(b1b_tile, NT, 3)
        P.barrier()

    if "B2" in phases:
        if "A2" not in phases or "B1a" not in phases:
            conv_flush()
            P.barrier()
        AR.reset()
        NB = 12
        GS = 4
        wq = AR.alloc([8, 2048], BF16)
        kTs = AR.alloc([16, 128], BF16)
        ident = AR.alloc([128], BF16)
        identf = AR.alloc([128])
        nfw_sb = AR.alloc([1024])
        nlw_sb = AR.alloc([1024])
        iota_sb = AR.alloc([128, 16], BF16)
        x1t = [AR.alloc([1024]) for _ in range(2)]
        hx = AR.alloc([1024])
        hbf = [AR.alloc([1024], BF16) for _ in range(2)]
        prod = [AR.alloc([1024], BF16) for _ in range(3)]
        junkD = AR.alloc([1024], BF16)
        junkA = AR.alloc([1024], BF16)
        hnT = AR.alloc([8, 128], BF16)
        qpT = AR.alloc([16, 128], BF16)
        s_all = AR.alloc([16, 128])
        work = AR.alloc([2048])
        s_w = work.rearrange("p (a b) -> p a b", b=128)
        cand_w = work.rearrange("p (a b) -> p a b", b=256)
        oh = work.rearrange("p (a b) -> p a b", b=16)
        vals = AR.alloc([16, 16])
        idxu = AR.alloc([16, 16], U32)
        i12f = AR.alloc([16, 16])
        cand = AR.alloc([8, 256])
        sc = AR.alloc([8, 16])
        flat = AR.alloc([8, 16], U32)
        fab = AR.alloc([8, 16], U32)
        fabf = AR.alloc([8, 16])
        e1f = AR.alloc([128])
        e2f = AR.alloc([128])
        idxf = AR.alloc([128])
        idx_i = [AR.alloc([128], I32) for _ in range(2)]
        es = AR.alloc([8, 16])
        ssum = AR.alloc([8])
        gsm = [AR.alloc([128]) for _ in range(2)]
        a_all = [AR.alloc([128]) for _ in range(2)]
        w_all = AR.alloc([128])
        w2 = AR.alloc([128])
        rs2 = AR.alloc([8])
        rs3 = AR.alloc([8])
        junk = AR.alloc([1024], BF16)
        junk2 = AR.alloc([1024])
        accv = AR.alloc([1024])
        dg = [AR.alloc([128], BF16) for _ in range(4)]
        gbuf = [AR.alloc([2048], BF16) for _ in range(NB)]
        for kc in range(8):
            dma("pool", wq[:, kc, :], peer_wq[kc * 128:(kc + 1) * 128, :], [], ["wq"], "wq")
        dma("pool", kTs, keysT.rearrange("p (a b) -> p a b", b=128), [], ["kTs"], "kTs")
        dma("sp", ident, c_ident, [], ["ident"], "identD")
        dma("sp", identf, c_identf, [], ["identf"], "identfD")
        dma("sp", nfw_sb, nfw, [], ["nfw"], "nfw")
        dma("sp", nlw_sb, nlw, [], ["nlw"], "nlw")
        dma("sp", iota_sb, c_iota.rearrange("p (a b) -> p a b", b=16), [], ["iota"], "iota")
        gcnt = [0]
        dcnt = [0]
        rrb2 = [0]
        if debug:
            print('B2 arena words', AR.off)

        def nbank2():
            bk = rrb2[0] % 4
            rrb2[0] += 1
            return bk, "ps%d" % bk

        def top16(src, wk, vout, iout, ksrc, kwork, kv, ki):
            P.op("dve", lambda e: e.max(out=vout[:, 0:8], in_=src), reads=[ksrc], writes=[kv])
            P.op("dve", lambda e: e.max_index(out=iout[:, 0:8], in_max=vout[:, 0:8], in_values=src),
                 reads=[ksrc, kv], writes=[ki])
            P.op("dve", lambda e: e.match_replace(out=wk, in_to_replace=vout[:, 0:8], in_values=src,
                                                  imm_value=-1e30), reads=[ksrc, kv], writes=[kwork])
            P.op("dve", lambda e: e.max(out=vout[:, 8:16], in_=wk), reads=[kwork], writes=[kv])
            P.op("dve", lambda e: e.max_index(out=iout[:, 8:16], in_max=vout[:, 8:16], in_values=wk),
                 reads=[kwork, kv], writes=[ki])

        def front(i):
            b = i % 2
            rows = slice(i * 128, (i + 1) * 128)
            kx, khx = "x1t%d" % b, "hx%d" % b
            dma("sp", x1t[b], x1s[rows, :], [], [kx], kx)
            rs = rs2[:, 0:1]
            stt(hx, x1t[b], 1.0, x1t[b], ALU.mult, ALU.mult, [kx], ["hxf", "rs2"], accum_out=rs)
            yield
            rstd_from_ss(rs, D, "rs2")
            yield
            stt(hbf[b], x1t[b], rs, nfw_sb, ALU.mult, ALU.mult, [kx, "rs2", "nfw"], [khx])
            yield
            bk, kb = nbank2()
            for kc in range(8):
                tr(psb16(bk)[:, kc * 128:(kc + 1) * 128], hbf[b][:, kc * 128:(kc + 1) * 128], ident, [khx, "ident"], [kb])
            cp("act", hnT, psb16(bk).rearrange("p (a b) -> p a b", b=128), [kb], ["hnT"])
            for grp in range(4):
                bk, kb = nbank2()
                for j in range(4):
                    ch = 4 * grp + j
                    for kc in range(8):
                        mm(psb(bk)[:, j * 128:(j + 1) * 128], wq[:, kc, ch * 128:(ch + 1) * 128], hnT[:, kc, :],
                           kc == 0, kc == 7, ["wq", "hnT"], [kb])
                cp("act", qpT[:, 4 * grp:4 * grp + 4, :], psb(bk).rearrange("p (a b) -> p a b", b=128), [kb], ["qpT"])
            for grp in range(4):
                bk, kb = nbank2()
                for j in range(4):
                    hc = 4 * grp + j
                    mm(psb(bk)[:, j * 128:(j + 1) * 128], qpT[:, hc, :], kTs[:, hc, :], True, True, ["qpT", "kTs"], [kb])
                cp("act", s_all[:, 4 * grp:4 * grp + 4, :], psb(bk).rearrange("p (a b) -> p a b", b=128), [kb], ["s_all"])
            for _sp in range(8):
                yield
            for hc in range(16):
                top16(s_all[:, hc, :], s_w[:, hc, :], vals[:, hc, :], idxu[:, hc, :], "s_all", "work", "vals", "idxu")
                yield
            cp("act", i12f, idxu, ["idxu"], ["i12f"])
            vals4 = vals.rearrange("p (h c) k -> p h c k", c=2)
            i12f4 = i12f.rearrange("p (h c) k -> p h c k", c=2)
            tt("dve", cand.rearrange("p h (a b) -> p h a b", b=16),
               bc(vals4[:, :, 0, :].unsqueeze(3), [128, 8, 16, 16]),
               bc(vals4[:, :, 1, :].unsqueeze(2), [128, 8, 16, 16]), ALU.add, ["vals"], ["cand"])
            yield
            for h in range(8):
                top16(cand[:, h, :], cand_w[:, h, :], sc[:, h, :], flat[:, h, :], "cand", "work", "sc", "flat")
                yield
            for which, (dstf, sh_op, sh_val) in enumerate([(e1f, ALU.logical_shift_right, 4), (e2f, ALU.bitwise_and, 15)]):
                ts("dve", fab, flat, sh_val, None, sh_op, None, ["flat"], ["fab"])
                cp("act", fabf, fab, ["fab"], ["fabf"])
                yield
                yield
                tt("dve", oh, iota_sb, bc(fabf.rearrange("p a b -> p (a b)").unsqueeze(2), [128, 128, 16]),
                   ALU.is_equal, ["iota", "fabf"], ["work"])
                yield
                oh4 = oh.rearrange("p (h k) a -> p h k a", k=16)
                tt("dve", oh4, oh4, bc(i12f4[:, :, which, :].unsqueeze(2), [128, 8, 16, 16]), ALU.mult,
                   ["work", "i12f"], ["work"])
                yield
                P.op("dve", lambda e, dstf=dstf: e.tensor_reduce(out=dstf, in_=oh, axis=AX.X, op=ALU.add),
                     reads=["work"], writes=["e12_%d" % which])
                yield
            stt(idxf, e1f, 128.0, e2f, ALU.mult, ALU.add, ["e12_0", "e12_1"], ["idxf"])
            cp("dve", idx_i[b], idxf, ["idxf"], ["idx%d" % b])
            tt("pool", es, sc, bc(sc[:, :, 0:1], [128, 8, 16]), ALU.subtract, ["sc"], ["es"])
            act(es, es, AF.Exp, ["es"], ["es"])
            yield
            yield
            P.op("dve", lambda e: e.tensor_reduce(out=ssum, in_=es, axis=AX.X, op=ALU.add), reads=["es"], writes=["ssum"])
            P.op("dve", lambda e: e.reciprocal(ssum, ssum), reads=["ssum"], writes=["ssum"])
            tt("pool", gsm[b].rearrange("p (a b) -> p a b", b=16), es, bc(ssum.unsqueeze(2), [128, 8, 16]), ALU.mult,
               ["es", "ssum"], ["gsm%d" % b])
            yield

        def back(i, tail_prev):
            b = i % 2
            kx, khx, kidx = "x1t%d" % b, "hx%d" % b, "idx%d" % b
            pa = 4 + 2 * b
            ngrp = 128 // GS
            pend = None

            def finish(j0, bufs):
                for jj, j in enumerate(range(j0, j0 + GS)):
                    bf, kgb = bufs[jj]
                    dk = dcnt[0] % 4
                    dcnt[0] += 1
                    act(w2[:, j:j + 1], w_all[:, j:j + 1], AF.Copy, ["w_all", "gsm%d" % b], ["w2"],
                        scale=gsm[b][:, j:j + 1])
                    act(dg[dk], identf, AF.Copy, ["identf", "w2"], ["dg%d" % dk], scale=w2[:, j:j + 1])
                    for half in range(2):
                        mm(psb(pa + half), dg[dk], gbuf[bf][:, 1024 + half * 512:1024 + (half + 1) * 512],
                           False, j == 127, ["dg%d" % dk, kgb], ["ps%d" % (pa + half)])

            for half in range(2):
                mm(psb(pa + half), identf, x1t[b][:, half * 512:(half + 1) * 512], True, False,
                   ["identf", kx], ["ps%d" % (pa + half)])
            for g_ in range(ngrp):
                j0 = g_ * GS
                bufs = []
                for j in range(j0, j0 + GS):
                    bf = gcnt[0] % NB
                    gcnt[0] += 1
                    kgb = "g%d" % bf
                    bufs.append((bf, kgb))
                    P.op("pool", lambda e, bf=bf, j=j, b=b: e.indirect_dma_start(
                        out=gbuf[bf], out_offset=None, in_=uvb,
                        in_offset=bass.IndirectOffsetOnAxis(ap=idx_i[b][:, j:j + 1], axis=0)),
                        reads=[kidx], writes=[kgb], dma=True, semkey=kgb)
                    if j % 4 == 0:
                        stt(junkD, gbuf[bf][:, 0:1024], 1.0, hbf[b], ALU.mult, ALU.mult, [kgb, khx],
                            ["junkD", "a_all"], accum_out=a_all[b][:, j:j + 1])
                    else:
                        pk = pcnt[0] % 3
                        pcnt[0] += 1
                        tt("dve", prod[pk], gbuf[bf][:, 0:1024], hbf[b], ALU.mult, [kgb, khx], ["prod%d" % pk])
                        act(junkA, prod[pk], AF.Copy, ["prod%d" % pk], ["junkA", "a_all"],
                            accum_out=a_all[b][:, j:j + 1])
                js = slice(j0, j0 + GS)
                act(w_all[:, js], a_all[b][:, js], AF.Gelu, ["a_all"], ["w_all"])
                if pend is not None:
                    finish(*pend)
                pend = (j0, bufs)
                if g_ == 2 and tail_prev is not None:
                    tail_prev()
                yield
            finish(*pend)

        def make_tail(i):
            b = i % 2
            rows = slice(i * 128, (i + 1) * 128)
            kx = "x1t%d" % b
            pa = 4 + 2 * b

            def tail():
                for half in range(2):
                    hs_ = slice(half * 512, (half + 1) * 512)
                    cp("act", accv[:, hs_], psb(pa + half), ["ps%d" % (pa + half)], ["accv"])
                rs = rs3[:, 0:1]
                stt(junk2, accv, 1.0, accv, ALU.mult, ALU.mult, ["accv"], ["junk2", "rs3"], accum_out=rs)
                rstd_from_ss(rs, D, "rs3")
                stt(junk2, accv, rs, nlw_sb, ALU.mult, ALU.mult, ["accv", "rs3", "nlw"], ["junk2"])
                dma("sp", out[rows, :], junk2, ["junk2"], [], "junk2")
            return tail

        pcnt = [0]
        for _ in front(0):
            pass
        tail_prev = None
        for i in range(NT):
            gf = front(i + 1) if i + 1 < NT else None
            for _ in back(i, tail_prev):
                if gf is not None:
                    for _k in range(3):
                        try:
                            next(gf)
                        except StopIteration:
                            gf = None
                            break
            if gf is not None:
                for _ in gf:
                    pass
            tail_prev = make_tail(i)
        tail_prev()
        P.barrier()

    outkeys = [k for k in P.dma_cnt]
    P.emit(out_semkeys=outkeys)
    st.close()
    return nc


def host_consts():
    bf = ml_dtypes.bfloat16
    k = np.arange(128)[:, None]
    q = np.arange(128)[None, :]
    mc = np.where(k <= q, 0.0, NEG).astype(np.float32)
    mp = np.where(k > q, 0.0, NEG).astype(np.float32)
    c = {
        "c_ident": np.eye(128, dtype=np.float32).astype(bf),
        "c_identf": np.eye(128, dtype=np.float32),
        "c_maskc": np.tile(mc, (1, 4)).astype(bf),
        "c_maskp": np.tile(mp, (1, 4)).astype(bf),
        "c_U": (k <= q).astype(np.float32),
        "c_L": (k > q).astype(np.float32),
        "c_ones": np.ones((128, 128), np.float32),
        "c_iota": np.tile(np.arange(16, dtype=np.float32), (128, 128)).astype(bf),
        "c_invf": np.tile((500000.0 ** (-np.arange(0, 16, 2, dtype=np.float32) / 16.0)).astype(np.float32),
                          (128, 1)),
    }
    return c


def rep(v, n=128):
    return np.ascontiguousarray(np.broadcast_to(np.asarray(v, np.float32).reshape(1, -1), (n, v.size)))


def make_in_maps(inputs, ncores=8):
    g = lambda k: np.asarray(inputs[k])
    shared = dict(host_consts())
    shared["w_in"] = np.ascontiguousarray(g("w_in")[0])
    shared["w_attn_o"] = np.ascontiguousarray(g("w_attn_o")[0])
    shared["w_ssd_o"] = np.ascontiguousarray(g("w_ssd_o")[0])
    shared["w_out"] = np.ascontiguousarray(g("w_out")[0])
    shared["peer_wq"] = np.ascontiguousarray(g("peer_wq")[0])
    shared["keysT"] = np.ascontiguousarray(g("peer_keys")[0].transpose(3, 0, 1, 2).reshape(128, 2048))
    shared["peer_uv"] = np.ascontiguousarray(
        np.concatenate([g("peer_u")[0], g("peer_v")[0]], axis=1))
    shared["nmw"] = rep(g("norm_mix_w")[0])
    shared["nfw"] = rep(g("norm_ffn_w")[0])
    shared["nlw"] = rep(g("norm_final_w"))
    shared["snwc"] = np.ascontiguousarray(g("ssd_norm_w")[0].reshape(16, 128).T)
    shared["sinks"] = rep(g("attn_sinks")[0])
    shared["convw"] = np.ascontiguousarray(g("conv_w")[0].reshape(4, 24, 128).transpose(2, 1, 0).reshape(128, 96))
    shared["convb"] = np.ascontiguousarray(g("conv_b")[0].reshape(24, 128).T)
    shared["dtb"] = rep(g("dt_bias")[0])
    shared["alog"] = rep(g("a_log")[0])
    shared["dskip"] = rep(g("d_skip")[0])
    maps = []
    xs = g("x")
    ps = g("positions")
    for c in range(ncores):
        m = dict(shared)
        m["x"] = np.ascontiguousarray(xs[c])
        m["pos"] = np.ascontiguousarray(ps[c].reshape(32, 128).T.astype(np.int32))
        maps.append(m)
    return maps


def kernel(**inputs):
    nc = build_program(32, debug=False)
    maps = make_in_maps(inputs, 8)
    res = run_bass_kernel_spmd(nc, maps, core_ids=list(range(8)))
    return np.stack([np.asarray(r["out"]) for r in res.results], axis=0).astype(np.float32)
```
